# Optimizing a Trainium2 kernel written in Bass

```python
import jax, jax.numpy as jnp
from jax import lax
import numpy as np

D_MODEL = 2048
BATCH = 8
SEQ = 2048
DEPTH = 2

GLA_HEADS = 4
GLA_DK = D_MODEL // 8
GLA_DV = D_MODEL // 4
GLA_KEY_W = GLA_HEADS * GLA_DK
GLA_VAL_W = GLA_HEADS * GLA_DV
GLA_GATE_RANK = 16
GLA_GATE_TAU = 16.0
GLA_CHUNK = 64
CONV_CH = D_MODEL
CONV_WIDTH = 31
FFN_HIDDEN = (8 * D_MODEL + 3 * 256 - 1) // (3 * 256) * 256
RMS_EPS = 1e-6
LN_EPS = 1e-5
IN_SPLITS = (GLA_KEY_W, GLA_KEY_W, GLA_VAL_W, GLA_VAL_W, GLA_GATE_RANK, 2 * CONV_CH, 2 * D_MODEL)
IN_W = sum(IN_SPLITS)

kernel_name = 'hybrid_gla_conformer_conv_gated_merge'


def _rmsnorm(x, g):
    x32 = x.astype(jnp.float32)
    y = x32 * lax.rsqrt(jnp.mean(x32 * x32, axis=-1, keepdims=True) + RMS_EPS)
    return (y * g.astype(jnp.float32)).astype(x.dtype)


def _layernorm(x, g, b):
    x32 = x.astype(jnp.float32)
    mu = jnp.mean(x32, axis=-1, keepdims=True)
    xc = x32 - mu
    y = xc * lax.rsqrt(jnp.mean(xc * xc, axis=-1, keepdims=True) + LN_EPS)
    return (y * g.astype(jnp.float32) + b.astype(jnp.float32)).astype(x.dtype)


def _split_cols(t, sizes):
    idx, acc = [], 0
    for s in sizes[:-1]:
        acc += s
        idx.append(acc)
    return jnp.split(t, idx, axis=-1)


def _gla_chunked(q, k, v, log_a):
    B, S, H, DK = q.shape
    DV = v.shape[-1]
    N = S // GLA_CHUNK

    def to_chunks(t):
        return t.astype(jnp.float32).reshape(B, N, GLA_CHUNK, H, t.shape[-1]).transpose(1, 0, 3, 2, 4)

    qc = to_chunks(q) * (GLA_DK ** -0.5)
    kc = to_chunks(k)
    vc = to_chunks(v)
    bc = jnp.cumsum(to_chunks(log_a), axis=3)
    causal = jnp.tril(jnp.ones((GLA_CHUNK, GLA_CHUNK), dtype=bool))

    def step(state, inp):
        q_c, k_c, v_c, b_c = inp
        qe = q_c * jnp.exp(b_c)
        ke = k_c * jnp.exp(-b_c)
        att = jnp.where(causal, jnp.einsum('bhcd,bhsd->bhcs', qe, ke), 0.0)
        o = jnp.einsum('bhcs,bhse->bhce', att, v_c) + jnp.einsum('bhcd,bhde->bhce', qe, state)
        b_last = b_c[:, :, -1:, :]
        kd = k_c * jnp.exp(b_last - b_c)
        new_state = jnp.exp(b_last[:, :, 0, :])[..., None] * state + jnp.einsum('bhcd,bhce->bhde', kd, v_c)
        return new_state, o

    s0 = jnp.zeros((B, H, DK, DV), jnp.float32)
    _, o = lax.scan(step, s0, (qc, kc, vc, bc))
    return o.transpose(1, 0, 3, 2, 4).reshape(B, S, H, DV)


def _mixer(h, w_in, w_alpha2, b_alpha2, gla_norm, w_o_gla, conv_w, conv_b,
           conv_norm_g, conv_norm_b, w_pw2, b_pw2, w_out):
    B, S, _ = h.shape
    proj = h @ w_in
    q, k, v, r, a_lr, glu_in, gates = _split_cols(proj, IN_SPLITS)

    z = (a_lr @ w_alpha2 + b_alpha2).astype(jnp.float32)
    log_a = jax.nn.log_sigmoid(z) / GLA_GATE_TAU
    o = _gla_chunked(q.reshape(B, S, GLA_HEADS, GLA_DK),
                     k.reshape(B, S, GLA_HEADS, GLA_DK),
                     v.reshape(B, S, GLA_HEADS, GLA_DV),
                     log_a.reshape(B, S, GLA_HEADS, GLA_DK))
    o = o * lax.rsqrt(jnp.mean(o * o, axis=-1, keepdims=True) + RMS_EPS)
    o = (o.reshape(B, S, GLA_VAL_W) * gla_norm.astype(jnp.float32)).astype(h.dtype)
    branch_a = (o * jax.nn.silu(r)) @ w_o_gla

    u_val, u_gate = jnp.split(glu_in, 2, axis=-1)
    u = u_val * jax.nn.sigmoid(u_gate)
    u = lax.conv_general_dilated(u, conv_w[:, None, :], window_strides=(1,),
                                 padding=[(CONV_WIDTH - 1, 0)],
                                 dimension_numbers=('NWC', 'WIO', 'NWC'),
                                 feature_group_count=CONV_CH) + conv_b
    u = jax.nn.silu(_layernorm(u, conv_norm_g, conv_norm_b))
    branch_b = u @ w_pw2 + b_pw2

    g_a, g_b = jnp.split(gates, 2, axis=-1)
    merged = jax.nn.sigmoid(g_a) * branch_a + jax.nn.sigmoid(g_b) * branch_b
    return merged @ w_out


def _swiglu(h, w_gate, w_up, w_down):
    return (jax.nn.silu(h @ w_gate) * (h @ w_up)) @ w_down


def setup_inputs(seed: int = 0) -> dict:
    key = jax.random.key(seed)
    ks = jax.random.split(key, 19)
    L = DEPTH

    def nrm(k, shape, scale):
        return jax.random.normal(k, shape, jnp.float32) * scale

    return {
        'x': nrm(ks[0], (BATCH, SEQ, D_MODEL), 1.0),
        'norm_mix': 1.0 + nrm(ks[1], (L, D_MODEL), 0.02),
        'w_in': nrm(ks[2], (L, D_MODEL, IN_W), D_MODEL ** -0.5),
        'w_alpha2': nrm(ks[3], (L, GLA_GATE_RANK, GLA_KEY_W), GLA_GATE_RANK ** -0.5),
        'b_alpha2': nrm(ks[4], (L, GLA_KEY_W), 0.02),
        'gla_norm': 1.0 + nrm(ks[5], (L, GLA_VAL_W), 0.02),
        'w_o_gla': nrm(ks[6], (L, GLA_VAL_W, D_MODEL), GLA_VAL_W ** -0.5),
        'conv_w': nrm(ks[7], (L, CONV_WIDTH, CONV_CH), CONV_WIDTH ** -0.5),
        'conv_b': nrm(ks[8], (L, CONV_CH), 0.02),
        'conv_norm_g': 1.0 + nrm(ks[9], (L, CONV_CH), 0.02),
        'conv_norm_b': nrm(ks[10], (L, CONV_CH), 0.02),
        'w_pw2': nrm(ks[11], (L, CONV_CH, D_MODEL), CONV_CH ** -0.5),
        'b_pw2': nrm(ks[12], (L, D_MODEL), 0.02),
        'w_out': nrm(ks[13], (L, D_MODEL, D_MODEL), D_MODEL ** -0.5),
        'norm_ffn': 1.0 + nrm(ks[14], (L, D_MODEL), 0.02),
        'w_gate': nrm(ks[15], (L, D_MODEL, FFN_HIDDEN), D_MODEL ** -0.5),
        'w_up': nrm(ks[16], (L, D_MODEL, FFN_HIDDEN), D_MODEL ** -0.5),
        'w_down': nrm(ks[17], (L, FFN_HIDDEN, D_MODEL), FFN_HIDDEN ** -0.5),
        'norm_final': 1.0 + nrm(ks[18], (D_MODEL,), 0.02),
    }


def reference(x, norm_mix, w_in, w_alpha2, b_alpha2, gla_norm, w_o_gla, conv_w, conv_b,
              conv_norm_g, conv_norm_b, w_pw2, b_pw2, w_out, norm_ffn, w_gate, w_up, w_down,
              norm_final):
    h = x
    for l in range(DEPTH):
        h = h + _mixer(_rmsnorm(h, norm_mix[l]), w_in[l], w_alpha2[l], b_alpha2[l], gla_norm[l],
                       w_o_gla[l], conv_w[l], conv_b[l], conv_norm_g[l], conv_norm_b[l],
                       w_pw2[l], b_pw2[l], w_out[l])
        h = h + _swiglu(_rmsnorm(h, norm_ffn[l]), w_gate[l], w_up[l], w_down[l])
    return _rmsnorm(h, norm_final)
```

```python
import numpy as np
import concourse.bass as bass
import concourse.mybir as mybir
from concourse.bass_utils import run_bass_kernel_spmd

F32 = mybir.dt.float32
BF16 = mybir.dt.bfloat16
AF = mybir.ActivationFunctionType
ALU = mybir.AluOpType

D = 2048
KC = 16
HID = 5632
HC = 44
NH = 4
DK = 256
DV = 512
CW = 31
RMS_EPS = 1e-6
LN_EPS = 1e-5
TAU = 16.0
OQ, OK_, OV, OR, OA, OUV, OUG, OGA, OGB = 0, 1024, 2048, 4096, 6144, 6160, 8208, 10256, 12304
V_NMIX, V_GLAN, V_CONVB, V_LNG, V_LNB, V_BPW2, V_NFFN, V_CW0 = 0, 1, 2, 3, 4, 5, 6, 7
NVEC = 7 + CW


class Buf:
    __slots__ = ("w", "r", "name", "psum")

    def __init__(self, name="", psum=False):
        self.w = {}
        self.r = {}
        self.name = name
        self.psum = psum


class Eng:
    def __init__(self, ctx, name, h):
        self.ctx, self.name, self.h = ctx, name, h
        self.sem = ctx.new_sem("e_" + name)
        self.cnt = 0
        self.seen = {}

    def wait(self, ev):
        sem, val = ev
        k = id(sem)
        if self.seen.get(k, 0) < val:
            self.h.wait_ge(sem, val)
            self.seen[k] = val

    def bump(self):
        if self.cnt >= 24000:
            self.sem = self.ctx.new_sem("e_" + self.name)
            self.cnt = 0
        self.cnt += 1
        return (self.sem, self.cnt)


class Slot:
    def __init__(self, ctx, name):
        self.ctx, self.name = ctx, name
        self.sem = ctx.new_sem("d_" + name)
        self.cnt = 0

    def bump(self):
        if self.cnt >= 24000:
            self.sem = self.ctx.new_sem("d_" + self.name)
            self.cnt = 0
        self.cnt += 16
        return (self.sem, self.cnt)


class Ctx:
    def __init__(self, nc, stack):
        self.nc = nc
        self.stack = stack
        self.nsem = 0
        self.pe = Eng(self, "pe", nc.tensor)
        self.act = Eng(self, "act", nc.scalar)
        self.dve = Eng(self, "dve", nc.vector)
        self.pool = Eng(self, "pool", nc.gpsimd)
        self.sp = Eng(self, "sp", nc.sync)
        self.engs = [self.pe, self.act, self.dve, self.pool, self.sp]
        self.slots = []
        self.slotmap = {}

    def new_sem(self, name):
        self.nsem += 1
        return self.stack.enter_context(self.nc.semaphore(f"{name}_{self.nsem}"))

    def slot(self, name):
        if name in self.slotmap:
            return self.slotmap[name]
        s = Slot(self, name)
        self.slots.append(s)
        self.slotmap[name] = s
        return s

    def _waits(self, eng, reads, writes, pwrites):
        own = id(eng.sem)
        for b in reads:
            for ev in b.w.values():
                eng.wait(ev)
            if b.psum:
                for k, ev in b.r.items():
                    if k != own:
                        eng.wait(ev)
        for b in writes:
            for k, ev in b.w.items():
                if k != own:
                    eng.wait(ev)
            for k, ev in b.r.items():
                if k != own:
                    eng.wait(ev)
        for b in pwrites:
            for k, ev in b.r.items():
                if k != own:
                    eng.wait(ev)

    @staticmethod
    def _record(ev, reads, writes, pwrites):
        k = id(ev[0])
        for b in reads:
            b.r[k] = ev
        for b in writes:
            b.w = {k: ev}
            b.r = {}
        for b in pwrites:
            b.w[k] = ev

    def op(self, eng, fn, reads=(), writes=(), pwrites=()):
        self._waits(eng, reads, writes, pwrites)
        inst = fn()
        ev = eng.bump()
        inst.then_inc(ev[0], 1)
        self._record(ev, reads, writes, pwrites)
        return ev

    def dma(self, q, slot, out, in_, reads=(), writes=(), pwrites=(), **kw):
        self._waits(q, reads, writes, pwrites)
        inst = q.h.dma_start(out=out, in_=in_, **kw)
        ev = slot.bump()
        inst.then_inc(ev[0], 16)
        self._record(ev, reads, writes, pwrites)
        return ev

    def barrier(self, engs=None):
        engs = engs or [self.pe, self.act, self.dve, self.sp]
        evs = [(e.sem, e.cnt) for e in self.engs if e.cnt > 0]
        evs += [(s.sem, s.cnt) for s in self.slots if s.cnt > 0]
        for e in engs:
            for ev in evs:
                if ev[0] is not e.sem:
                    e.wait(ev)


class Arena:
    def __init__(self, nc, name, nbytes):
        self.t = nc.alloc_sbuf_tensor("sb_" + name, [128, nbytes // 4], F32)
        self.nbytes = nbytes
        self.off = 0

    def reset(self, off=0):
        self.off = off

    def view(self, off, shape, dtype, parts=128):
        esz = 2 if dtype == BF16 else 4
        n = 1
        for s in shape[1:]:
            n *= s
        nb = n * esz
        assert off % 4 == 0 and nb % 4 == 0 and off + nb <= self.nbytes, (off, nb, self.nbytes)
        ap = self.t[0:shape[0], off // 4:(off + nb) // 4]
        if dtype != F32:
            ap = ap.bitcast(dtype)
        if len(shape) == 3:
            ap = ap.rearrange("p (a b) -> p a b", a=shape[1])
        elif len(shape) == 4:
            ap = ap.rearrange("p (a b c) -> p a b c", a=shape[1], b=shape[2])
        return ap

    def alloc(self, shape, dtype):
        esz = 2 if dtype == BF16 else 4
        n = 1
        for s in shape[1:]:
            n *= s
        nb = (n * esz + 31) // 32 * 32
        v = self.view(self.off, shape, dtype)
        self.off += nb
        assert self.off <= self.nbytes, (self.off, self.nbytes)
        return v


def build_program(T, L, final_norm=True, dbg=None):
    from contextlib import ExitStack
    assert T % 512 == 0
    NT = T // 128
    TG = T // 512
    nc = bass.Bass("TRN2", target_bir_lowering=False)
    dt_ = nc.dram_tensor
    x_d = dt_("x", [T, D], F32, kind="ExternalInput").ap()
    w_in = dt_("w_in", [L, D, 14352], F32, kind="ExternalInput").ap()
    wa2 = dt_("wa2aug", [L, 17, 1024], F32, kind="ExternalInput").ap()
    w_og = dt_("w_o_gla", [L, D, D], F32, kind="ExternalInput").ap()
    w_pw2 = dt_("w_pw2", [L, D, D], F32, kind="ExternalInput").ap()
    w_out = dt_("w_out", [L, D, D], F32, kind="ExternalInput").ap()
    w_gate = dt_("w_gate", [L, D, HID], F32, kind="ExternalInput").ap()
    w_up = dt_("w_up", [L, D, HID], F32, kind="ExternalInput").ap()
    w_down = dt_("w_down", [L, HID, D], F32, kind="ExternalInput").ap()
    vecs_d = dt_("vecs", [L, 128, KC * NVEC], F32, kind="ExternalInput").ap()
    nfin_d = dt_("nfin", [1, D], F32, kind="ExternalInput").ap()
    cst_d = dt_("cst", [3, 128, 128], F32, kind="ExternalInput").ap()
    y_d = dt_("y", [T, D], F32, kind="ExternalOutput").ap()
    hA = dt_("hA", [T, D], F32).ap()
    hB = dt_("hB", [T, D], F32).ap()
    og_d = dt_("og_d", [D, T], BF16).ap()
    siga_d = dt_("siga_d", [D, T], BF16).ap()
    sigb_d = dt_("sigb_d", [D, T], BF16).ap()
    tb_d = dt_("tb_d", [D, T], BF16).ap()
    act_d = dt_("act_d", [T // 256, 128, HC, 256], BF16).ap()
    dbg_d = {}
    if dbg:
        for name, shape in dbg.items():
            dbg_d[name] = dt_("dbg_" + name, shape, F32, kind="ExternalOutput").ap()

    stack = ExitStack()
    cx = Ctx(nc, stack)
    pe, act, dve, pool, sp = cx.pe, cx.act, cx.dve, cx.pool, cx.sp

    ABYTES = 65536
    assert KC * T * 2 <= ABYTES
    WBYTES = 2 * KC * 512 * 2
    big = Arena(nc, "big", ABYTES + WBYTES + ABYTES)
    OFF_A, OFF_WB, OFF_A2 = 0, ABYTES, ABYTES + WBYTES
    misc = Arena(nc, "misc", 40 * 1024)
    cst = Arena(nc, "cst", 6 * 1024)

    A = big.view(OFF_A, [128, KC, T], BF16)
    A2 = big.view(OFF_A2, [128, KC, T], BF16)
    WB = [big.view(OFF_WB + i * KC * 512 * 2, [128, KC, 512], BF16) for i in range(2)]
    bAm = [[Buf(f"A{c}_{g}") for g in range(TG)] for c in range(KC)]
    bA = [b for row in bAm for b in row]

    def bAt(tg):
        return [bAm[c][tg] for c in range(KC)]
    bA2 = [Buf(f"A2_{c}") for c in range(KC)]
    bWB = [Buf("wb0"), Buf("wb1")]
    wslot = [cx.slot("w0"), cx.slot("w1")]
    wctr = [0]

    psb = [nc.alloc_psum_tensor(f"ps{i}", [128, 512], F32) for i in range(8)]
    bps = [Buf(f"ps{i}", psum=True) for i in range(8)]
    psc = [0]

    def psum():
        i = psc[0] % 8
        psc[0] += 1
        return psb[i], bps[i]

    identf = cst.alloc([128, 128], F32)
    trif = cst.alloc([128, 128], F32)
    upf = cst.alloc([128, 128], F32)
    identb = cst.alloc([128, 128], BF16)
    onesb = cst.alloc([128, 128], BF16)
    vecs = cst.alloc([128, KC, NVEC], F32)
    bcst = Buf("cst")
    bvecs = Buf("vecs")
    cslot = cx.slot("c")
    cx.dma(sp, cslot, identf, cst_d[0], writes=[bcst])
    cx.dma(sp, cslot, trif, cst_d[1], pwrites=[bcst])
    cx.dma(sp, cslot, upf, cst_d[2], pwrites=[bcst])
    cx.op(dve, lambda: nc.vector.tensor_copy(out=identb, in_=identf), reads=[bcst], pwrites=[bcst])
    cx.op(dve, lambda: nc.vector.memset(onesb, 1.0), pwrites=[bcst])

    def vcol(c, j):
        return vecs[:, c, j:j + 1]

    def load_panel(pieces):
        i = wctr[0] % 2
        wctr[0] += 1
        first = True
        for src, co in pieces:
            n = src.shape[1]
            cx.dma(pool, wslot[i], WB[i][:, :, co:co + n], src.rearrange("(c p) n -> p c n", p=128),
                   writes=[bWB[i]] if first else (), pwrites=() if first else [bWB[i]])
            first = False
        return WB[i], bWB[i]

    def mm_group(out_ap, pairs, reads, bout):
        def emit():
            n = len(pairs)
            inst = None
            for i, (l, r) in enumerate(pairs):
                inst = nc.tensor.matmul(out_ap, lhsT=l, rhs=r, start=(i == 0), stop=(i == n - 1))
            return inst
        return cx.op(pe, emit, reads=reads, writes=[bout])

    def norm_to_A(src, gcol):
        cx.barrier()
        misc.reset()
        ht = [big.view(OFF_A2 + i * D * 4, [128, D], F32) for i in range(8)]
        yb = [misc.alloc([128, D], BF16) for _ in range(4)]
        junk = misc.alloc([128, D], BF16)
        st = misc.alloc([128, 2, 8], F32)
        bht = [Buf() for _ in range(8)]
        byb = [Buf() for _ in range(4)]
        bjunk = Buf()
        bst = [Buf(), Buf()]
        hs = [cx.slot(f"nh{i}") for i in range(8)]

        def load(g):
            for j in range(4):
                s_ = (g % 2) * 4 + j
                i = g * 4 + j
                cx.dma(sp, hs[s_], ht[s_], src[i * 128:(i + 1) * 128, :], writes=[bht[s_]])

        def stats(g):
            p = g % 2
            for j in range(4):
                s_ = p * 4 + j
                cx.op(act, lambda: nc.scalar.activation(out=junk, in_=ht[s_], func=AF.Square, accum_out=st[:, p, j:j + 1]),
                      reads=[bht[s_]], writes=[bjunk] if j == 0 else (), pwrites=[bst[p]] + ([] if j == 0 else [bjunk]))
            cx.op(act, lambda: nc.scalar.activation(out=st[:, p, 4:8], in_=st[:, p, 0:4], func=AF.Sqrt, scale=1.0 / D, bias=epsr),
                  reads=[bst[p], bcst], pwrites=[bst[p]])
            cx.op(dve, lambda: nc.vector.reciprocal(out=st[:, p, 4:8], in_=st[:, p, 4:8]), reads=[bst[p]], pwrites=[bst[p]])

        def scale(g):
            p = g % 2
            for j in range(4):
                s_ = p * 4 + j
                rs = st[:, p, 4 + j:5 + j]
                if j % 2 == 0:
                    cx.op(dve, lambda: nc.vector.tensor_scalar(out=yb[j], in0=ht[s_], scalar1=rs, scalar2=None, op0=ALU.mult),
                          reads=[bht[s_], bst[p]], writes=[byb[j]])
                else:
                    cx.op(act, lambda: nc.scalar.activation(out=yb[j], in_=ht[s_], func=AF.Copy, scale=rs),
                          reads=[bht[s_], bst[p]], writes=[byb[j]])

        def trans(g):
            for c in range(KC):
                pt, bp = psum()
                ptb = pt[:].bitcast(BF16)

                def emit():
                    inst = None
                    for j in range(4):
                        inst = nc.tensor.transpose(ptb[:, j * 128:(j + 1) * 128], yb[j][:, c * 128:(c + 1) * 128], identb)
                    return inst
                cx.op(pe, emit, reads=byb + [bcst], writes=[bp])
                cx.op(dve, lambda: nc.vector.tensor_scalar(out=A[:, c, g * 512:(g + 1) * 512], in0=ptb[:, 0:512],
                                                           scalar1=vcol(c, gcol), scalar2=None, op0=ALU.mult),
                      reads=[bp, bvecs], writes=[bAm[c][g]])

        load(0)
        if TG > 1:
            load(1)
        stats(0)
        for g in range(TG):
            if g + 1 < TG:
                stats(g + 1)
            scale(g)
            trans(g)
            if g + 2 < TG:
                load(g + 2)
        return bht

    def final_norm_out(src):
        cx.barrier()
        misc.reset()
        ht = [big.view(OFF_A2 + i * D * 4, [128, D], F32) for i in range(8)]
        ot = [big.view(OFF_A + i * D * 4, [128, D], F32) for i in range(8)]
        gb = misc.alloc([128, D], F32)
        junk = misc.alloc([128, D], BF16)
        st = misc.alloc([128, 2, 8], F32)
        bgb, bjunk = Buf(), Buf()
        bht = [Buf() for _ in range(8)]
        bot = [Buf() for _ in range(8)]
        bst = [Buf(), Buf()]
        hs = [cx.slot(f"fh{i}") for i in range(8)]
        os_ = [cx.slot(f"fo{i}") for i in range(8)]
        cx.dma(sp, cslot, gb, nfin_d[0:1, :].broadcast_to([128, D]), writes=[bgb])

        def load(g):
            for j in range(4):
                s_ = (g % 2) * 4 + j
                i = g * 4 + j
                cx.dma(sp, hs[s_], ht[s_], src[i * 128:(i + 1) * 128, :], writes=[bht[s_]])

        def stats(g):
            p = g % 2
            for j in range(4):
                s_ = p * 4 + j
                cx.op(act, lambda: nc.scalar.activation(out=junk, in_=ht[s_], func=AF.Square, accum_out=st[:, p, j:j + 1]),
                      reads=[bht[s_]], writes=[bjunk] if j == 0 else (), pwrites=[bst[p]] + ([] if j == 0 else [bjunk]))
            cx.op(act, lambda: nc.scalar.activation(out=st[:, p, 4:8], in_=st[:, p, 0:4], func=AF.Sqrt, scale=1.0 / D, bias=epsr),
                  reads=[bst[p], bcst], pwrites=[bst[p]])
            cx.op(dve, lambda: nc.vector.reciprocal(out=st[:, p, 4:8], in_=st[:, p, 4:8]), reads=[bst[p]], pwrites=[bst[p]])

        def scale(g):
            p = g % 2
            for j in range(4):
                s_ = p * 4 + j
                i = g * 4 + j
                cx.op(dve, lambda: nc.vector.scalar_tensor_tensor(out=ot[s_], in0=ht[s_], scalar=st[:, p, 4 + j:5 + j], in1=gb,
                                                                  op0=ALU.mult, op1=ALU.mult),
                      reads=[bht[s_], bst[p], bgb], writes=[bot[s_]])
                cx.dma(sp, os_[s_], y_d[i * 128:(i + 1) * 128, :], ot[s_], reads=[bot[s_]])

        load(0)
        if TG > 1:
            load(1)
        stats(0)
        for g in range(TG):
            if g + 1 < TG:
                stats(g + 1)
            scale(g)
            if g + 2 < TG:
                load(g + 2)

    epsr = cst.alloc([128, 1], F32)
    epsl = cst.alloc([128, 1], F32)
    cx.op(dve, lambda: nc.vector.memset(epsr, RMS_EPS), pwrites=[bcst])
    cx.op(dve, lambda: nc.vector.memset(epsl, LN_EPS), pwrites=[bcst])

    def dump(name, sb_ap, bufs):
        if name in dbg_d:
            cx.barrier()
            s = cx.slot("dbg")
            ev = cx.dma(sp, s, dbg_d[name], sb_ap, reads=bufs)
            sp.wait(ev)

    for l in range(L):
        hsrc = x_d if l == 0 else hA
        cx.barrier()
        cx.dma(sp, cslot, vecs, vecs_d[l].rearrange("p (c v) -> p c v", c=KC), writes=[bvecs])

        norm_to_A(hsrc, V_NMIX)
        if l == 0 and "A0" in dbg_d:
            cx.barrier()
            misc.reset()
            tmp = misc.alloc([128, 2048], F32)
            btmp = Buf()
            s = cx.slot("dbg")
            for c in range(KC):
                for t0 in range(0, T, 2048):
                    n = min(2048, T - t0)
                    cx.op(dve, lambda: nc.vector.tensor_copy(out=tmp[:, 0:n], in_=A[:, c, t0:t0 + n]), reads=bAm[c], writes=[btmp])
                    cx.dma(sp, s, dbg_d["A0"][:, c * T + t0:c * T + t0 + n], tmp[:, 0:n], reads=[btmp])

        cx.barrier()
        misc.reset()
        alr = misc.alloc([32, T], F32)
        wa2s = misc.alloc([32, 256], F32)
        balr, bwa2 = Buf(), Buf()
        MISC_GLA0 = misc.off
        cx.op(dve, lambda: nc.vector.memset(alr, 1.0), writes=[balr])
        wp, bw = load_panel([(w_in[l][:, OA:OA + 16], 0)])
        for tg in range(TG):
            pt, bp = psum()
            mm_group(pt[0:16, :], [(wp[:, c, 0:16], A[:, c, tg * 512:(tg + 1) * 512]) for c in range(KC)],
                     reads=[bw] + bA, bout=bp)
            cx.op(act, lambda: nc.scalar.copy(out=alr[0:16, tg * 512:(tg + 1) * 512], in_=pt[0:16, :]),
                  reads=[bp], pwrites=[balr])

        qe = big.view(OFF_A2, [128, 2, T], BF16)
        ke = big.view(OFF_A2 + 4 * T, [128, 2, T], BF16)
        kd = big.view(OFF_A2 + 8 * T, [128, NT, 256], BF16)
        vT = big.view(OFF_A2 + 8 * T + NT * 512, [128, NT, 512], BF16)
        srT = big.view(OFF_A2 + 8 * T + NT * 1536, [128, NT, 512], BF16)
        OGF0 = 8 * T + NT * 2560
        assert OGF0 <= ABYTES
        ogF_in_A2 = OGF0 + 2 * 4096 <= ABYTES
        if ogF_in_A2:
            ogF = [big.view(OFF_A2 + OGF0 + i * 4096, [128, 4, 512], BF16) for i in range(2)]
        bqe, bke, bkd, bvT, bsrT = Buf(), Buf(), Buf(), Buf(), Buf()
        misc.reset(MISC_GLA0)
        e_t = [misc.alloc([128, 2, 256], F32) for _ in range(2)]
        la = misc.alloc([128, 4, 256], F32)
        ebp = misc.alloc([128, 2, 512], F32)
        ebn = misc.alloc([128, 2, 512], F32)
        edk = misc.alloc([128, 4, 256], F32)
        kraw = misc.alloc([128, 2, 512], BF16)
        bkraw = Buf()
        dec = misc.alloc([128, 2, NT], F32)
        S = [misc.alloc([128, 2, 512], BF16) for _ in range(2)]
        attb = [misc.alloc([128, 128], BF16) for _ in range(2)]
        osq = misc.alloc([128, 512], BF16)
        ost = misc.alloc([128, 8], F32)
        ogT = [misc.alloc([128, 512], BF16) for _ in range(2)]
        if not ogF_in_A2:
            ogF = [misc.alloc([128, 4, 512], BF16) for _ in range(2)]
        be_t, bla, bebp, bebn, bedk, bdec = [Buf(), Buf()], Buf(), Buf(), Buf(), Buf(), Buf()
        bS, battb, bosq, bost, bogT, bogF = [Buf(), Buf()], [Buf(), Buf()], Buf(), [Buf(), Buf()], [Buf(), Buf()], [Buf(), Buf()]
        ogs = [cx.slot("og0"), cx.slot("og1")]
        bog_d = Buf()
        og_v = og_d.rearrange("(c p) t -> p c t", p=128)
        blk = 0
        for hd in range(NH):
            cx.dma(sp, cslot, wa2s[0:17, :], wa2[l][:, hd * 256:(hd + 1) * 256], writes=[bwa2])
            wp, bw = load_panel([(w_in[l][:, OQ + hd * DK:OQ + (hd + 1) * DK], 0),
                                 (w_in[l][:, OK_ + hd * DK:OK_ + (hd + 1) * DK], 256)])
            for tg in range(TG):
                tsl = slice(tg * 512, (tg + 1) * 512)
                for jj in range(2):
                    pt, bp = psum()

                    def emit():
                        inst = None
                        for j2 in range(2):
                            i = tg * 4 + jj * 2 + j2
                            inst = nc.tensor.matmul(pt[:, j2 * 256:(j2 + 1) * 256], lhsT=alr[0:17, i * 128:(i + 1) * 128],
                                                    rhs=wa2s[0:17, 0:256], start=True, stop=True)
                        return inst
                    cx.op(pe, emit, reads=[balr, bwa2], writes=[bp])
                    et = e_t[jj]
                    cx.op(act, lambda: nc.scalar.activation(out=et, in_=pt[:].rearrange("p (a b) -> p a b", a=2), func=AF.Exp, scale=-1.0),
                          reads=[bp], writes=[be_t[jj]])
                    cx.op(act, lambda: nc.scalar.activation(out=la[:, jj * 2:jj * 2 + 2, :], in_=et, func=AF.Ln, bias=1.0),
                          reads=[be_t[jj]], pwrites=[bla])
                for fc in range(2):
                    pt, bp = psum()

                    def emit():
                        inst = None
                        for j in range(4):
                            inst = nc.tensor.matmul(pt[:, j * 128:(j + 1) * 128], lhsT=la[:, j, fc * 128:(fc + 1) * 128],
                                                    rhs=trif, start=True, stop=True)
                        return inst
                    cx.op(pe, emit, reads=[bla, bcst], writes=[bp])
                    cx.op(act, lambda: nc.scalar.activation(out=ebp[:, fc, :], in_=pt[:], func=AF.Exp, scale=-1.0 / TAU),
                          reads=[bp], pwrites=[bebp])
                    cx.op(act, lambda: nc.scalar.activation(out=ebn[:, fc, :], in_=pt[:], func=AF.Exp, scale=1.0 / TAU),
                          reads=[bp], pwrites=[bebn])
                    cx.op(dve, lambda: nc.vector.tensor_copy(out=dec[:, fc, tg * 4:(tg + 1) * 4],
                                                             in_=ebp[:, fc, :].rearrange("p (j t) -> p j t", j=4)[:, :, 127]),
                          reads=[bebp], pwrites=[bdec])
                for jj in range(2):
                    pt, bp = psum()

                    def emit():
                        inst = None
                        for j2 in range(2):
                            inst = nc.tensor.matmul(pt[:, j2 * 256:(j2 + 1) * 256], lhsT=upf, rhs=la[:, jj * 2 + j2, :],
                                                    start=True, stop=True)
                        return inst
                    cx.op(pe, emit, reads=[bla, bcst], writes=[bp])
                    cx.op(act, lambda: nc.scalar.activation(out=edk[:, jj * 2:jj * 2 + 2, :], in_=pt[:].rearrange("p (a b) -> p a b", a=2),
                                                            func=AF.Exp, scale=-1.0 / TAU),
                          reads=[bp], pwrites=[bedk])
                for fc in range(2):
                    pt, bp = psum()
                    mm_group(pt[:], [(wp[:, c, fc * 128:(fc + 1) * 128], A[:, c, tsl]) for c in range(KC)], reads=[bw] + bA, bout=bp)
                    cx.op(dve, lambda: nc.vector.scalar_tensor_tensor(out=qe[:, fc, tsl], in0=pt[:], scalar=float(DK) ** -0.5,
                                                                      in1=ebp[:, fc, :], op0=ALU.mult, op1=ALU.mult),
                          reads=[bp, bebp], pwrites=[bqe])
                    pt, bp = psum()
                    mm_group(pt[:], [(wp[:, c, 256 + fc * 128:256 + (fc + 1) * 128], A[:, c, tsl]) for c in range(KC)], reads=[bw] + bA, bout=bp)
                    cx.op(dve, lambda: nc.vector.tensor_tensor(out=ke[:, fc, tsl], in0=pt[:], in1=ebn[:, fc, :], op=ALU.mult),
                          reads=[bp, bebn], pwrites=[bke])
                    cx.op(act, lambda: nc.scalar.copy(out=kraw[:, fc, :], in_=pt[:]), reads=[bp], pwrites=[bkraw])
                for jj in range(2):
                    pt, bp = psum()
                    ptb = pt[:].bitcast(BF16)

                    def emit():
                        inst = None
                        for j2 in range(2):
                            j = jj * 2 + j2
                            for fc in range(2):
                                inst = nc.tensor.transpose(ptb[:, j2 * 256 + fc * 128:j2 * 256 + (fc + 1) * 128],
                                                           kraw[:, fc, j * 128:(j + 1) * 128], identb)
                        return inst
                    cx.op(pe, emit, reads=[bkraw, bcst], writes=[bp])
                    i0_ = tg * 4 + jj * 2
                    cx.op(dve, lambda: nc.vector.tensor_tensor(out=kd[:, i0_:i0_ + 2, :], in0=ptb[:, 0:512].rearrange("p (a b) -> p a b", a=2),
                                                               in1=edk[:, jj * 2:jj * 2 + 2, :], op=ALU.mult),
                          reads=[bp, bedk], pwrites=[bkd])
            wp, bw = load_panel([(w_in[l][:, OV + hd * DV:OV + (hd + 1) * DV], 0)])
            for i in range(NT):
                pt, bp = psum()
                mm_group(pt[:], [(A[:, c, i * 128:(i + 1) * 128], wp[:, c, :]) for c in range(KC)], reads=[bw] + bA, bout=bp)
                cx.op(act, lambda: nc.scalar.copy(out=vT[:, i, :], in_=pt[:]), reads=[bp], pwrites=[bvT])
            wp, bw = load_panel([(w_in[l][:, OR + hd * DV:OR + (hd + 1) * DV], 0)])
            for i in range(NT):
                pt, bp = psum()
                mm_group(pt[:], [(A[:, c, i * 128:(i + 1) * 128], wp[:, c, :]) for c in range(KC)], reads=[bw] + bA, bout=bp)
                cx.op(act, lambda: nc.scalar.activation(out=srT[:, i, :], in_=pt[:], func=AF.Silu), reads=[bp], pwrites=[bsrT])
            rec = {}

            def r_att(n):
                nsl = slice(n * 128, (n + 1) * 128)
                pa, bpa = psum()
                mm_group(pa[:, 0:128], [(ke[:, fc, nsl], qe[:, fc, nsl]) for fc in range(2)], reads=[bke, bqe], bout=bpa)
                ab, bab = attb[n % 2], battb[n % 2]
                cx.op(dve, lambda: nc.vector.tensor_tensor(out=ab, in0=pa[:, 0:128], in1=trif, op=ALU.mult),
                      reads=[bpa, bcst], writes=[bab])

            def r_main(n):
                nsl = slice(n * 128, (n + 1) * 128)
                sc, sn = S[n % 2], S[(n + 1) % 2]
                bsc, bsn = bS[n % 2], bS[(n + 1) % 2]
                ab, bab = attb[n % 2], battb[n % 2]
                pss = []
                if n < NT - 1:
                    for fc in range(2):
                        p2, bp2 = psum()
                        mm_group(p2[:], [(kd[:, n, fc * 128:(fc + 1) * 128], vT[:, n, :])], reads=[bkd, bvT], bout=bp2)
                        pss.append((p2, bp2))
                po, bpo = psum()
                pairs = [(ab, vT[:, n, :])]
                rd = [bab, bvT]
                if n > 0:
                    pairs += [(qe[:, fc, nsl], sc[:, fc, :]) for fc in range(2)]
                    rd += [bqe, bsc]
                mm_group(po[:], pairs, reads=rd, bout=bpo)
                if n < NT - 1:
                    for fc in range(2):
                        p2, bp2 = pss[fc]
                        if n == 0:
                            cx.op(dve, lambda: nc.vector.tensor_copy(out=sn[:, fc, :], in_=p2[:]), reads=[bp2], pwrites=[bsn])
                        else:
                            cx.op(dve, lambda: nc.vector.scalar_tensor_tensor(out=sn[:, fc, :], in0=sc[:, fc, :], scalar=dec[:, fc, n:n + 1],
                                                                              in1=p2[:], op0=ALU.mult, op1=ALU.add),
                                  reads=[bp2, bsc, bdec], pwrites=[bsn])
                ss, rs = ost[:, 2 * (n % 2):2 * (n % 2) + 1], ost[:, 2 * (n % 2) + 1:2 * (n % 2) + 2]
                bo = bost[n % 2]
                cx.op(act, lambda: nc.scalar.activation(out=osq, in_=po[:], func=AF.Square, accum_out=ss),
                      reads=[bpo], writes=[bosq, bo])
                cx.op(act, lambda: nc.scalar.activation(out=rs, in_=ss, func=AF.Sqrt, scale=1.0 / DV, bias=epsr),
                      reads=[bo, bcst], pwrites=[bo])
                cx.op(dve, lambda: nc.vector.reciprocal(out=rs, in_=rs), reads=[bo], pwrites=[bo])
                og, bog = ogT[n % 2], bogT[n % 2]
                cx.op(dve, lambda: nc.vector.scalar_tensor_tensor(out=og, in0=po[:], scalar=rs, in1=srT[:, n, :],
                                                                  op0=ALU.mult, op1=ALU.mult),
                      reads=[bpo, bo, bsrT], writes=[bog])

            def r_tr(n, blk):
                og, bog = ogT[n % 2], bogT[n % 2]
                ptt, bpt = psum()
                ptb = ptt[:].bitcast(BF16)

                def emit():
                    inst = None
                    for ec in range(4):
                        inst = nc.tensor.transpose(ptb[:, ec * 128:(ec + 1) * 128], og[:, ec * 128:(ec + 1) * 128], identb)
                    return inst
                cx.op(pe, emit, reads=[bog, bcst], writes=[bpt])
                g = (blk // 4) % 2
                for ec in range(4):
                    cx.op(act, lambda: nc.scalar.activation(out=ogF[g][:, ec, (n % 4) * 128:(n % 4 + 1) * 128],
                                                            in_=ptb[:, ec * 128:(ec + 1) * 128], func=AF.Copy,
                                                            scale=vcol(hd * 4 + ec, V_GLAN)),
                          reads=[bpt, bvecs], pwrites=[bogF[g]])
                if n % 4 == 3:
                    tg = n // 4
                    cx.dma(sp, ogs[g], og_v[:, hd * 4:(hd + 1) * 4, tg * 512:(tg + 1) * 512], ogF[g],
                           reads=[bogF[g]], pwrites=[bog_d])

            r_att(0)
            for n in range(NT):
                if n + 1 < NT:
                    r_att(n + 1)
                r_main(n)
                if n >= 1:
                    r_tr(n - 1, blk + n - 1)
            r_tr(NT - 1, blk + NT - 1)
            blk += NT
        if l == 0 and "og" in dbg_d:
            cx.barrier()
            misc.reset()
            tb16 = misc.alloc([128, 2048], BF16)
            tmp = misc.alloc([128, 2048], F32)
            btmp, bt16 = Buf(), Buf()
            s = cx.slot("dbg")
            s2 = cx.slot("dbg2")
            for c in range(KC):
                cx.dma(sp, s2, tb16[:, 0:T], og_v[:, c, :], reads=[bog_d], writes=[bt16])
                cx.op(dve, lambda: nc.vector.tensor_copy(out=tmp[:, 0:T], in_=tb16[:, 0:T]), reads=[bt16], writes=[btmp])
                cx.dma(sp, s, dbg_d["og"][:, c * T:(c + 1) * T], tmp[:, 0:T], reads=[btmp])

        cx.barrier()
        misc.reset()
        PADL = 32
        ub = [misc.alloc([128, PADL + T], BF16) for _ in range(2)]
        NDV = 8
        dg = [misc.alloc([128, CW - NDV, 128], BF16) for _ in range(2)]
        sg = [misc.alloc([128, 512], F32) for _ in range(2)]
        accd = misc.alloc([128, T], F32)
        baccd = Buf()
        bub, bdg, bsg = [Buf(), Buf()], [Buf(), Buf()], [Buf(), Buf()]
        for s_ in range(2):
            cx.op(dve, lambda: nc.vector.memset(ub[s_][:, 0:PADL], 0.0), pwrites=[bub[s_]])
        sgk = [0]
        cpan = {}

        def glu(cc):
            cp, c2 = cc // 2, cc % 2
            if c2 == 0:
                cpan[cp] = load_panel([(w_in[l][:, OUV + cp * 256:OUV + (cp + 1) * 256], 0),
                                       (w_in[l][:, OUG + cp * 256:OUG + (cp + 1) * 256], 256)])
            wp, bw = cpan[cp]
            u, bu = ub[cc % 2], bub[cc % 2]
            for tg in range(TG):
                tsl = slice(tg * 512, (tg + 1) * 512)
                pg, bpg = psum()
                mm_group(pg[:], [(wp[:, c, 256 + c2 * 128:256 + (c2 + 1) * 128], A[:, c, tsl]) for c in range(KC)], reads=[bw] + bA, bout=bpg)
                pv, bpv = psum()
                mm_group(pv[:], [(wp[:, c, c2 * 128:(c2 + 1) * 128], A[:, c, tsl]) for c in range(KC)], reads=[bw] + bA, bout=bpv)
                sgt, bsgt = sg[sgk[0] % 2], bsg[sgk[0] % 2]
                sgk[0] += 1
                cx.op(act, lambda: nc.scalar.activation(out=sgt, in_=pg[:], func=AF.Sigmoid), reads=[bpg], writes=[bsgt])
                cx.op(dve, lambda: nc.vector.tensor_tensor(out=u[:, PADL + tg * 512:PADL + (tg + 1) * 512], in0=pv[:], in1=sgt, op=ALU.mult),
                      reads=[bpv, bsgt], pwrites=[bu])
            d_, bd_ = dg[cc % 2], bdg[cc % 2]
            for j in range(NDV, CW):
                cx.op(act, lambda: nc.scalar.activation(out=d_[:, j - NDV, :], in_=identb, func=AF.Copy, scale=vcol(cc, V_CW0 + j)),
                      reads=[bcst, bvecs], pwrites=[bd_])

        def conv_dve(cc):
            u, bu = ub[cc % 2], bub[cc % 2]
            for j in range(NDV):
                src = u[:, PADL - (CW - 1) + j:PADL - (CW - 1) + j + T]
                wj = vcol(cc, V_CW0 + j)
                if j == 0:
                    cx.op(dve, lambda: nc.vector.tensor_scalar(out=accd, in0=src, scalar1=wj, scalar2=vcol(cc, V_CONVB),
                                                               op0=ALU.mult, op1=ALU.add),
                          reads=[bu, bvecs], writes=[baccd])
                else:
                    cx.op(dve, lambda: nc.vector.scalar_tensor_tensor(out=accd, in0=src, scalar=wj, in1=accd, op0=ALU.mult, op1=ALU.add),
                          reads=[bu, bvecs], pwrites=[baccd])

        def conv_pe(cc):
            u, bu = ub[cc % 2], bub[cc % 2]
            d_, bd_ = dg[cc % 2], bdg[cc % 2]
            for tg in range(TG):
                pc, bpc = psum()
                mm_group(pc[:], [(d_[:, j - NDV, :], u[:, PADL - (CW - 1) + j + tg * 512:PADL - (CW - 1) + j + (tg + 1) * 512])
                                 for j in range(NDV, CW)],
                         reads=[bu, bd_], bout=bpc)
                cx.op(dve, lambda: nc.vector.tensor_tensor(out=A2[:, cc, tg * 512:(tg + 1) * 512], in0=pc[:],
                                                           in1=accd[:, tg * 512:(tg + 1) * 512], op=ALU.add),
                      reads=[bpc, baccd], pwrites=[bA2[cc]])

        glu(0)
        for cc in range(KC):
            conv_dve(cc)
            if cc + 1 < KC:
                glu(cc + 1)
            conv_pe(cc)

        misc.reset(misc.off)
        gst = [misc.alloc([128, 512], BF16) for _ in range(4)]
        bgst = [Buf() for _ in range(4)]
        gss = [cx.slot(f"gs{i}") for i in range(4)]
        bsig = [Buf("siga"), Buf("sigb")]
        gk = 0
        for which, (off, dst) in enumerate(((OGA, siga_d), (OGB, sigb_d))):
            for pn in range(4):
                wp, bw = load_panel([(w_in[l][:, off + pn * 512:off + (pn + 1) * 512], 0)])
                for fc in range(4):
                    for tg in range(TG):
                        tsl = slice(tg * 512, (tg + 1) * 512)
                        pt, bp = psum()
                        mm_group(pt[:], [(wp[:, c, fc * 128:(fc + 1) * 128], A[:, c, tsl]) for c in range(KC)], reads=[bw] + bA, bout=bp)
                        s = gk % 4
                        gk += 1
                        cx.op(act, lambda: nc.scalar.activation(out=gst[s], in_=pt[:], func=AF.Sigmoid), reads=[bp], writes=[bgst[s]])
                        r0 = (pn * 4 + fc) * 128
                        cx.dma(sp, gss[s], dst[r0:r0 + 128, tsl], gst[s], reads=[bgst[s]], pwrites=[bsig[which]])

        cx.barrier()
        misc.reset()
        sq = [misc.alloc([128, 512], BF16) for _ in range(4)]
        mean = [misc.alloc([128, 512], F32) for _ in range(2)]
        rstd = [misc.alloc([128, 512], F32) for _ in range(2)]
        t1 = [misc.alloc([128, 512], F32) for _ in range(4)]
        bsq, bmean, brstd, bt1 = [Buf() for _ in range(4)], [Buf(), Buf()], [Buf(), Buf()], [Buf() for _ in range(4)]
        ogs2 = cx.slot("ogl")
        for c in range(KC):
            cx.dma(sp, ogs2, A[:, c, :], og_d[c * 128:(c + 1) * 128, :], reads=[bog_d], writes=bAm[c])

        def ln_stats(tg):
            tsl = slice(tg * 512, (tg + 1) * 512)
            mn, rs_, bmn, brs = mean[tg % 2], rstd[tg % 2], bmean[tg % 2], brstd[tg % 2]
            p1, bp1 = psum()
            mm_group(p1[:], [(onesb, A2[:, c, tsl]) for c in range(KC)], reads=bA2 + [bcst], bout=bp1)
            p2, bp2 = psum()
            for c in range(KC):
                s_ = c % 4
                cx.op(pool, lambda: nc.gpsimd.tensor_tensor(out=sq[s_], in0=A2[:, c, tsl], in1=A2[:, c, tsl], op=ALU.mult),
                      reads=[bA2[c]], writes=[bsq[s_]])
                cx.op(pe, lambda: nc.tensor.matmul(p2[:], lhsT=onesb, rhs=sq[s_], start=(c == 0), stop=(c == KC - 1)),
                      reads=[bsq[s_], bcst], writes=[bp2] if c == 0 else (), pwrites=() if c == 0 else [bp2])
            cx.op(act, lambda: nc.scalar.mul(out=mn, in_=p1[:], mul=1.0 / D), reads=[bp1], writes=[bmn])
            cx.op(dve, lambda: nc.vector.tensor_tensor(out=rs_, in0=mn, in1=mn, op=ALU.mult), reads=[bmn], writes=[brs])
            cx.op(dve, lambda: nc.vector.scalar_tensor_tensor(out=rs_, in0=p2[:], scalar=1.0 / D, in1=rs_, op0=ALU.mult, op1=ALU.subtract),
                  reads=[bp2, brs], pwrites=[brs])
            cx.op(act, lambda: nc.scalar.activation(out=rs_, in_=rs_, func=AF.Sqrt, bias=epsl), reads=[brs, bcst], pwrites=[brs])
            cx.op(dve, lambda: nc.vector.reciprocal(out=rs_, in_=rs_), reads=[brs], pwrites=[brs])

        def ln_apply(tg):
            tsl = slice(tg * 512, (tg + 1) * 512)
            mn, rs_, bmn, brs = mean[tg % 2], rstd[tg % 2], bmean[tg % 2], brstd[tg % 2]
            for c in range(KC):
                s_ = c % 4
                cx.op(dve, lambda: nc.vector.tensor_tensor(out=t1[s_], in0=A2[:, c, tsl], in1=mn, op=ALU.subtract),
                      reads=[bA2[c], bmn], writes=[bt1[s_]])
                cx.op(dve, lambda: nc.vector.tensor_tensor(out=t1[s_], in0=t1[s_], in1=rs_, op=ALU.mult),
                      reads=[bt1[s_], brs], pwrites=[bt1[s_]])
                cx.op(act, lambda: nc.scalar.activation(out=A2[:, c, tsl], in_=t1[s_], func=AF.Silu,
                                                        scale=vcol(c, V_LNG), bias=vcol(c, V_LNB)),
                      reads=[bt1[s_], bvecs], pwrites=[bA2[c]])

        pan3 = {pn: load_panel([(w_pw2[l][:, pn * 512:(pn + 1) * 512], 0)]) for pn in range(2)}
        ln_stats(0)
        for tg in range(TG):
            if tg + 1 < TG:
                ln_stats(tg + 1)
            ln_apply(tg)
        if l == 0 and "ua" in dbg_d:
            cx.barrier()
            misc.reset()
            tmp = misc.alloc([128, 2048], F32)
            btmp = Buf()
            s = cx.slot("dbg")
            for c in range(KC):
                cx.op(dve, lambda: nc.vector.tensor_copy(out=tmp[:, 0:T], in_=A2[:, c, :]), reads=[bA2[c]], writes=[btmp])
                cx.dma(sp, s, dbg_d["ua"][:, c * T:(c + 1) * T], tmp[:, 0:T], reads=[btmp])

        def run_pipe(n, ld, body, pd):
            for k_ in range(min(pd, n)):
                ld(k_)
            for k_ in range(n):
                if k_ + pd < n:
                    ld(k_ + pd)
                body(k_)

        RG, PD = 4, 3
        cx.barrier()
        misc.reset()
        sgi = [misc.alloc([128, 512], BF16) for _ in range(RG)]
        tbo = [misc.alloc([128, 512], BF16) for _ in range(RG)]
        bsgi, btbo = [Buf() for _ in range(RG)], [Buf() for _ in range(RG)]
        sgis = [cx.slot(f"si{i}") for i in range(RG)]
        tbos = [cx.slot(f"to{i}") for i in range(RG)]
        btb_d = Buf("tb_d")
        tasks = [(pn, fc, tg) for pn in range(4) for fc in range(4) for tg in range(TG)]
        pan = dict(pan3)

        def ld3(k):
            pn, fc, tg = tasks[k]
            ch = pn * 4 + fc
            s_ = k % RG
            cx.dma(sp, sgis[s_], sgi[s_], sigb_d[ch * 128:(ch + 1) * 128, tg * 512:(tg + 1) * 512], reads=[bsig[1]], writes=[bsgi[s_]])

        def body3(k):
            pn, fc, tg = tasks[k]
            if pn not in pan:
                pan[pn] = load_panel([(w_pw2[l][:, pn * 512:(pn + 1) * 512], 0)])
            wp, bw = pan[pn]
            ch = pn * 4 + fc
            tsl = slice(tg * 512, (tg + 1) * 512)
            s_ = k % RG
            pt, bp = psum()
            mm_group(pt[:], [(wp[:, c, fc * 128:(fc + 1) * 128], A2[:, c, tsl]) for c in range(KC)], reads=[bw] + bA2, bout=bp)
            cx.op(dve, lambda: nc.vector.scalar_tensor_tensor(out=tbo[s_], in0=pt[:], scalar=vcol(ch, V_BPW2), in1=sgi[s_],
                                                              op0=ALU.add, op1=ALU.mult),
                  reads=[bp, bsgi[s_], bvecs], writes=[btbo[s_]])
            cx.dma(sp, tbos[s_], tb_d[ch * 128:(ch + 1) * 128, tsl], tbo[s_], reads=[btbo[s_]], pwrites=[btb_d])
        run_pipe(len(tasks), ld3, body3, PD)

        misc.reset(misc.off)
        sai = [misc.alloc([128, 512], BF16) for _ in range(RG)]
        tbi = [misc.alloc([128, 512], BF16) for _ in range(RG)]
        m1 = [misc.alloc([128, 512], F32) for _ in range(2)]
        bsai, btbi, bm1 = [Buf() for _ in range(RG)], [Buf() for _ in range(RG)], [Buf(), Buf()]
        sais = [cx.slot(f"sa{i}") for i in range(RG)]
        tbis = [cx.slot(f"ti{i}") for i in range(RG)]
        pan = {}

        def ld2(k):
            pn, fc, tg = tasks[k]
            ch = pn * 4 + fc
            s_ = k % RG
            tsl = slice(tg * 512, (tg + 1) * 512)
            cx.dma(sp, sais[s_], sai[s_], siga_d[ch * 128:(ch + 1) * 128, tsl], reads=[bsig[0]], writes=[bsai[s_]])
            cx.dma(sp, tbis[s_], tbi[s_], tb_d[ch * 128:(ch + 1) * 128, tsl], reads=[btb_d], writes=[btbi[s_]])

        def body2(k):
            pn, fc, tg = tasks[k]
            if pn not in pan:
                pan[pn] = load_panel([(w_og[l][:, pn * 512:(pn + 1) * 512], 0)])
            wp, bw = pan[pn]
            ch = pn * 4 + fc
            tsl = slice(tg * 512, (tg + 1) * 512)
            s_ = k % RG
            s2 = k % 2
            pt, bp = psum()
            mm_group(pt[:], [(wp[:, c, fc * 128:(fc + 1) * 128], A[:, c, tsl]) for c in range(KC)], reads=[bw] + bA, bout=bp)
            cx.op(dve, lambda: nc.vector.tensor_tensor(out=m1[s2], in0=pt[:], in1=sai[s_], op=ALU.mult),
                  reads=[bp, bsai[s_]], writes=[bm1[s2]])
            cx.op(dve, lambda: nc.vector.tensor_tensor(out=A2[:, ch, tsl], in0=m1[s2], in1=tbi[s_], op=ALU.add),
                  reads=[bm1[s2], btbi[s_]], pwrites=[bA2[ch]])
        run_pipe(len(tasks), ld2, body2, PD)

        misc.reset(misc.off)
        hi = [misc.alloc([128, 512], F32) for _ in range(RG)]
        ho = [misc.alloc([128, 512], F32) for _ in range(RG)]
        bhi, bho = [Buf() for _ in range(RG)], [Buf() for _ in range(RG)]
        his = [cx.slot(f"hi{i}") for i in range(RG)]
        hos = [cx.slot(f"ho{i}") for i in range(RG)]
        bhB = Buf("hB")
        bhA = Buf("hA")
        tasks4 = [(pn, i) for pn in range(4) for i in range(NT)]
        pan = {}

        def ld4(k):
            pn, i = tasks4[k]
            s_ = k % RG
            cx.dma(sp, his[s_], hi[s_], hsrc[i * 128:(i + 1) * 128, pn * 512:(pn + 1) * 512], writes=[bhi[s_]])

        def body4(k):
            pn, i = tasks4[k]
            if pn not in pan:
                pan[pn] = load_panel([(w_out[l][:, pn * 512:(pn + 1) * 512], 0)])
            wp, bw = pan[pn]
            s_ = k % RG
            pt, bp = psum()
            mm_group(pt[:], [(A2[:, c, i * 128:(i + 1) * 128], wp[:, c, :]) for c in range(KC)], reads=[bw] + bA2, bout=bp)
            cx.op(dve, lambda: nc.vector.tensor_tensor(out=ho[s_], in0=pt[:], in1=hi[s_], op=ALU.add),
                  reads=[bp, bhi[s_]], writes=[bho[s_]])
            cx.dma(sp, hos[s_], hB[i * 128:(i + 1) * 128, pn * 512:(pn + 1) * 512], ho[s_], reads=[bho[s_]], pwrites=[bhB])
        run_pipe(len(tasks4), ld4, body4, PD)

        nbht = norm_to_A(hB, V_NFFN)

        misc.reset(misc.off)
        W7 = [big.view(OFF_A2, [128, HC, 512], BF16), big.view(OFF_A, [128, HC, 512], BF16)]
        assert HC * 512 * 2 + 2 * HC * 256 * 2 <= ABYTES + WBYTES
        AT = [big.view(OFF_A + HC * 512 * 2 + i * HC * 256 * 2, [128, HC, 256], BF16) for i in range(2)]
        bW7, bAT = [Buf(), Buf()], [Buf(), Buf()]
        w7s = [cx.slot("w70"), cx.slot("w71")]
        ats = [cx.slot("at0"), cx.slot("at1")]

        def load_w7(pn, extra=()):
            i = pn % 2
            half = HC // 2
            for hh in range(2):
                cx.dma(pool, w7s[i], W7[i][:, hh * half:(hh + 1) * half, :],
                       w_down[l][hh * half * 128:(hh + 1) * half * 128, pn * 512:(pn + 1) * 512].rearrange("(c p) n -> p c n", p=128),
                       writes=([bW7[i]] + list(extra)) if hh == 0 else (), pwrites=() if hh == 0 else [bW7[i]])
        sgf = [misc.alloc([128, 512], F32) for _ in range(2)]
        ao = [misc.alloc([128, 512], BF16) for _ in range(3)]
        bsgf, bao = [Buf(), Buf()], [Buf() for _ in range(3)]
        aos = [cx.slot(f"ao{i}") for i in range(3)]
        bact_d = Buf("act_d")
        k3 = 0
        for pn in range(HC // 2):
            wp, bw = load_panel([(w_gate[l][:, pn * 256:(pn + 1) * 256], 0), (w_up[l][:, pn * 256:(pn + 1) * 256], 256)])
            if pn == 3:
                load_w7(0, extra=nbht)
            for fc in range(2):
                hc = pn * 2 + fc
                for tg in range(TG):
                    tsl = slice(tg * 512, (tg + 1) * 512)
                    s = k3 % 3
                    s2 = k3 % 2
                    k3 += 1
                    pg, bpg = psum()
                    mm_group(pg[:], [(wp[:, c, fc * 128:(fc + 1) * 128], A[:, c, tsl]) for c in range(KC)], reads=[bw] + bAt(tg), bout=bpg)
                    pu, bpu = psum()
                    mm_group(pu[:], [(wp[:, c, 256 + fc * 128:256 + (fc + 1) * 128], A[:, c, tsl]) for c in range(KC)], reads=[bw] + bAt(tg), bout=bpu)
                    cx.op(act, lambda: nc.scalar.activation(out=sgf[s2], in_=pg[:], func=AF.Silu), reads=[bpg], writes=[bsgf[s2]])
                    cx.op(dve, lambda: nc.vector.tensor_tensor(out=ao[s], in0=pu[:], in1=sgf[s2], op=ALU.mult),
                          reads=[bpu, bsgf[s2]], writes=[bao[s]])
                    cx.dma(sp, aos[s], act_d[tg * 2:tg * 2 + 2, :, hc, :].rearrange("a p t -> p a t"),
                           ao[s].rearrange("p (a t) -> p a t", a=2), reads=[bao[s]], pwrites=[bact_d])

        cx.barrier(engs=cx.engs)
        misc.reset()
        hi = [misc.alloc([128, 512], F32) for _ in range(RG)]
        ho = [misc.alloc([128, 512], F32) for _ in range(RG)]
        bhi, bho = [Buf() for _ in range(RG)], [Buf() for _ in range(RG)]
        his = [cx.slot(f"hi{i}") for i in range(RG)]
        hos = [cx.slot(f"ho{i}") for i in range(RG)]

        NTT = T // 256
        tiles = [(pn, tt) for pn in range(4) for tt in range(NTT)]
        tasks7 = [(pn, tt, sub) for (pn, tt) in tiles for sub in range(2)]

        def load_at(idx):
            a = idx % 2
            cx.dma(sp, ats[a], AT[a], act_d[tiles[idx][1]], reads=[bact_d], writes=[bAT[a]])

        def ld7(k):
            pn, tt, sub = tasks7[k]
            i = tt * 2 + sub
            s_ = k % RG
            cx.dma(sp, his[s_], hi[s_], hB[i * 128:(i + 1) * 128, pn * 512:(pn + 1) * 512], reads=[bhB], writes=[bhi[s_]])

        def body7(k):
            pn, tt, sub = tasks7[k]
            idx = k // 2
            if sub == 0:
                if tt == 0 and pn + 1 < 4:
                    load_w7(pn + 1)
                if idx + 1 < len(tiles):
                    load_at(idx + 1)
            wv, bwv = W7[pn % 2], bW7[pn % 2]
            a = idx % 2
            i = tt * 2 + sub
            s_ = k % RG
            pt, bp = psum()
            mm_group(pt[:], [(AT[a][:, c, sub * 128:(sub + 1) * 128], wv[:, c, :]) for c in range(HC)], reads=[bwv, bAT[a]], bout=bp)
            cx.op(dve, lambda: nc.vector.tensor_tensor(out=ho[s_], in0=pt[:], in1=hi[s_], op=ALU.add),
                  reads=[bp, bhi[s_]], writes=[bho[s_]])
            cx.dma(sp, hos[s_], hA[i * 128:(i + 1) * 128, pn * 512:(pn + 1) * 512], ho[s_], reads=[bho[s_]], pwrites=[bhA])

        load_at(0)
        run_pipe(len(tasks7), ld7, body7, PD)
        cx.barrier(engs=cx.engs)

    if final_norm:
        final_norm_out(hA)
    else:
        pass
    cx.barrier(engs=[sp])
    return nc, stack


def _host_layout(inputs, L):
    f = lambda a: np.ascontiguousarray(np.asarray(a, dtype=np.float32))
    rows = []
    for l in range(L):
        r = [inputs["norm_mix"][l], inputs["gla_norm"][l], inputs["conv_b"][l], inputs["conv_norm_g"][l],
             inputs["conv_norm_b"][l], inputs["b_pw2"][l], inputs["norm_ffn"][l]]
        r += [inputs["conv_w"][l][j] for j in range(CW)]
        m = np.stack([np.asarray(v, dtype=np.float32) for v in r], axis=0)
        m = m.reshape(NVEC, KC, 128).transpose(2, 1, 0)
        rows.append(m.reshape(128, KC * NVEC))
    vecs = f(np.stack(rows, axis=0))
    wa2aug = f(np.concatenate([np.asarray(inputs["w_alpha2"], dtype=np.float32),
                               np.asarray(inputs["b_alpha2"], dtype=np.float32)[:, None, :]], axis=1))
    ident = np.eye(128, dtype=np.float32)
    tri = np.triu(np.ones((128, 128), dtype=np.float32))
    upper = np.tril(np.ones((128, 128), dtype=np.float32), -1)
    cstm = f(np.stack([ident, tri, upper], axis=0))
    return vecs, wa2aug, cstm


_CACHE = {}


def kernel(**inputs):
    x = np.asarray(inputs["x"], dtype=np.float32)
    B, T, _ = x.shape
    L = inputs["w_in"].shape[0]
    key = (T, L)
    if key not in _CACHE:
        _CACHE[key] = build_program(T, L)
    nc, _stack = _CACHE[key]
    vecs, wa2aug, cstm = _host_layout(inputs, L)
    f = lambda a: np.ascontiguousarray(np.asarray(a, dtype=np.float32))
    shared = {
        "w_in": f(inputs["w_in"]), "wa2aug": wa2aug, "w_o_gla": f(inputs["w_o_gla"]), "w_pw2": f(inputs["w_pw2"]),
        "w_out": f(inputs["w_out"]), "w_gate": f(inputs["w_gate"]), "w_up": f(inputs["w_up"]), "w_down": f(inputs["w_down"]),
        "vecs": vecs, "nfin": f(inputs["norm_final"]).reshape(1, D), "cst": cstm,
    }
    in_maps = [dict(shared, x=np.ascontiguousarray(x[b])) for b in range(B)]
    res = run_bass_kernel_spmd(nc, in_maps, core_ids=list(range(B)))
    return np.stack([np.asarray(r["y"], dtype=np.float32) for r in res.results], axis=0)
```

```python
import numpy as np
import concourse.bass as bass
import concourse.mybir as mybir
from concourse.bass_utils import run_bass_kernel_spmd

F32 = mybir.dt.float32
BF16 = mybir.dt.bfloat16
AF = mybir.ActivationFunctionType
ALU = mybir.AluOpType

D = 2048
KC = 16
HID = 5632
HC = 44
NH = 4
DK = 256
DV = 512
CW = 31
RMS_EPS = 1e-6
LN_EPS = 1e-5
TAU = 16.0
OQ, OK_, OV, OR, OA, OUV, OUG, OGA, OGB = 0, 1024, 2048, 4096, 6144, 6160, 8208, 10256, 12304
V_NMIX, V_GLAN, V_CONVB, V_LNG, V_LNB, V_BPW2, V_NFFN, V_CW0 = 0, 1, 2, 3, 4, 5, 6, 7
NVEC = 7 + CW


class Buf:
    __slots__ = ("w", "r", "name", "psum")

    def __init__(self, name="", psum=False):
        self.w = {}
        self.r = {}
        self.name = name
        self.psum = psum


class Eng:
    def __init__(self, ctx, name, h):
        self.ctx, self.name, self.h = ctx, name, h
        self.sem = ctx.new_sem("e_" + name)
        self.cnt = 0
        self.seen = {}

    def wait(self, ev):
        sem, val = ev
        k = id(sem)
        if self.seen.get(k, 0) < val:
            self.h.wait_ge(sem, val)
            self.seen[k] = val

    def bump(self):
        if self.cnt >= 24000:
            self.sem = self.ctx.new_sem("e_" + self.name)
            self.cnt = 0
        self.cnt += 1
        return (self.sem, self.cnt)


class Slot:
    def __init__(self, ctx, name):
        self.ctx, self.name = ctx, name
        self.sem = ctx.new_sem("d_" + name)
        self.cnt = 0

    def bump(self):
        if self.cnt >= 24000:
            self.sem = self.ctx.new_sem("d_" + self.name)
            self.cnt = 0
        self.cnt += 16
        return (self.sem, self.cnt)


class Ctx:
    def __init__(self, nc, stack):
        self.nc = nc
        self.stack = stack
        self.nsem = 0
        self.pe = Eng(self, "pe", nc.tensor)
        self.act = Eng(self, "act", nc.scalar)
        self.dve = Eng(self, "dve", nc.vector)
        self.pool = Eng(self, "pool", nc.gpsimd)
        self.sp = Eng(self, "sp", nc.sync)
        self.engs = [self.pe, self.act, self.dve, self.pool, self.sp]
        self.slots = []
        self.slotmap = {}

    def new_sem(self, name):
        self.nsem += 1
        return self.stack.enter_context(self.nc.semaphore(f"{name}_{self.nsem}"))

    def slot(self, name):
        if name in self.slotmap:
            return self.slotmap[name]
        s = Slot(self, name)
        self.slots.append(s)
        self.slotmap[name] = s
        return s

    def _waits(self, eng, reads, writes, pwrites):
        own = id(eng.sem)
        for b in reads:
            for ev in b.w.values():
                eng.wait(ev)
            if b.psum:
                for k, ev in b.r.items():
                    if k != own:
                        eng.wait(ev)
        for b in writes:
            for k, ev in b.w.items():
                if k != own:
                    eng.wait(ev)
            for k, ev in b.r.items():
                if k != own:
                    eng.wait(ev)
        for b in pwrites:
            for k, ev in b.r.items():
                if k != own:
                    eng.wait(ev)

    @staticmethod
    def _record(ev, reads, writes, pwrites):
        k = id(ev[0])
        for b in reads:
            b.r[k] = ev
        for b in writes:
            b.w = {k: ev}
            b.r = {}
        for b in pwrites:
            b.w[k] = ev

    def op(self, eng, fn, reads=(), writes=(), pwrites=()):
        self._waits(eng, reads, writes, pwrites)
        inst = fn()
        ev = eng.bump()
        inst.then_inc(ev[0], 1)
        self._record(ev, reads, writes, pwrites)
        return ev

    def dma(self, q, slot, out, in_, reads=(), writes=(), pwrites=(), **kw):
        self._waits(q, reads, writes, pwrites)
        inst = q.h.dma_start(out=out, in_=in_, **kw)
        ev = slot.bump()
        inst.then_inc(ev[0], 16)
        self._record(ev, reads, writes, pwrites)
        return ev

    def barrier(self, engs=None):
        engs = engs or [self.pe, self.act, self.dve, self.sp]
        evs = [(e.sem, e.cnt) for e in self.engs if e.cnt > 0]
        evs += [(s.sem, s.cnt) for s in self.slots if s.cnt > 0]
        for e in engs:
            for ev in evs:
                if ev[0] is not e.sem:
                    e.wait(ev)


class Arena:
    def __init__(self, nc, name, nbytes):
        self.t = nc.alloc_sbuf_tensor("sb_" + name, [128, nbytes // 4], F32)
        self.nbytes = nbytes
        self.off = 0

    def reset(self, off=0):
        self.off = off

    def view(self, off, shape, dtype, parts=128):
        esz = 2 if dtype == BF16 else 4
        n = 1
        for s in shape[1:]:
            n *= s
        nb = n * esz
        assert off % 4 == 0 and nb % 4 == 0 and off + nb <= self.nbytes, (off, nb, self.nbytes)
        ap = self.t[0:shape[0], off // 4:(off + nb) // 4]
        if dtype != F32:
            ap = ap.bitcast(dtype)
        if len(shape) == 3:
            ap = ap.rearrange("p (a b) -> p a b", a=shape[1])
        elif len(shape) == 4:
            ap = ap.rearrange("p (a b c) -> p a b c", a=shape[1], b=shape[2])
        return ap

    def alloc(self, shape, dtype):
        esz = 2 if dtype == BF16 else 4
        n = 1
        for s in shape[1:]:
            n *= s
        nb = (n * esz + 31) // 32 * 32
        v = self.view(self.off, shape, dtype)
        self.off += nb
        assert self.off <= self.nbytes, (self.off, self.nbytes)
        return v


def build_program(T, L, final_norm=True, dbg=None):
    from contextlib import ExitStack
    assert T % 512 == 0
    NT = T // 128
    TG = T // 512
    nc = bass.Bass("TRN2", target_bir_lowering=False)
    dt_ = nc.dram_tensor
    x_d = dt_("x", [T, D], F32, kind="ExternalInput").ap()
    w_in = dt_("w_in", [L, D, 14352], F32, kind="ExternalInput").ap()
    wa2 = dt_("wa2aug", [L, 17, 1024], F32, kind="ExternalInput").ap()
    w_og = dt_("w_o_gla", [L, D, D], F32, kind="ExternalInput").ap()
    w_pw2 = dt_("w_pw2", [L, D, D], F32, kind="ExternalInput").ap()
    w_out = dt_("w_out", [L, D, D], F32, kind="ExternalInput").ap()
    w_gate = dt_("w_gate", [L, D, HID], F32, kind="ExternalInput").ap()
    w_up = dt_("w_up", [L, D, HID], F32, kind="ExternalInput").ap()
    w_down = dt_("w_down", [L, HID, D], F32, kind="ExternalInput").ap()
    vecs_d = dt_("vecs", [L, 128, KC * NVEC], F32, kind="ExternalInput").ap()
    nfin_d = dt_("nfin", [1, D], F32, kind="ExternalInput").ap()
    cst_d = dt_("cst", [3, 128, 128], F32, kind="ExternalInput").ap()
    y_d = dt_("y", [T, D], F32, kind="ExternalOutput").ap()
    hA = dt_("hA", [T, D], F32).ap()
    hB = dt_("hB", [T, D], F32).ap()
    og_d = dt_("og_d", [D, T], BF16).ap()
    siga_d = dt_("siga_d", [D, T], BF16).ap()
    sigb_d = dt_("sigb_d", [D, T], BF16).ap()
    tb_d = dt_("tb_d", [D, T], BF16).ap()
    act_d = dt_("act_d", [T // 256, 128, HC, 256], BF16).ap()
    dbg_d = {}
    if dbg:
        for name, shape in dbg.items():
            dbg_d[name] = dt_("dbg_" + name, shape, F32, kind="ExternalOutput").ap()

    stack = ExitStack()
    cx = Ctx(nc, stack)
    pe, act, dve, pool, sp = cx.pe, cx.act, cx.dve, cx.pool, cx.sp

    ABYTES = 65536
    assert KC * T * 2 <= ABYTES
    WBYTES = 2 * KC * 512 * 2
    big = Arena(nc, "big", ABYTES + WBYTES + ABYTES)
    OFF_A, OFF_WB, OFF_A2 = 0, ABYTES, ABYTES + WBYTES
    misc = Arena(nc, "misc", 40 * 1024)
    cst = Arena(nc, "cst", 6 * 1024)

    A = big.view(OFF_A, [128, KC, T], BF16)
    A2 = big.view(OFF_A2, [128, KC, T], BF16)
    WB = [big.view(OFF_WB + i * KC * 512 * 2, [128, KC, 512], BF16) for i in range(2)]
    bAm = [[Buf(f"A{c}_{g}") for g in range(TG)] for c in range(KC)]
    bA = [b for row in bAm for b in row]

    def bAt(tg):
        return [bAm[c][tg] for c in range(KC)]
    bA2 = [Buf(f"A2_{c}") for c in range(KC)]
    bWB = [Buf("wb0"), Buf("wb1")]
    wslot = [cx.slot("w0"), cx.slot("w1")]
    wctr = [0]

    psb = [nc.alloc_psum_tensor(f"ps{i}", [128, 512], F32) for i in range(8)]
    bps = [Buf(f"ps{i}", psum=True) for i in range(8)]
    psc = [0]

    def psum():
        i = psc[0] % 8
        psc[0] += 1
        return psb[i], bps[i]

    identf = cst.alloc([128, 128], F32)
    trif = cst.alloc([128, 128], F32)
    upf = cst.alloc([128, 128], F32)
    identb = cst.alloc([128, 128], BF16)
    onesb = cst.alloc([128, 128], BF16)
    vecs = cst.alloc([128, KC, NVEC], F32)
    bcst = Buf("cst")
    bvecs = Buf("vecs")
    cslot = cx.slot("c")
    cx.dma(sp, cslot, identf, cst_d[0], writes=[bcst])
    cx.dma(sp, cslot, trif, cst_d[1], pwrites=[bcst])
    cx.dma(sp, cslot, upf, cst_d[2], pwrites=[bcst])
    cx.op(dve, lambda: nc.vector.tensor_copy(out=identb, in_=identf), reads=[bcst], pwrites=[bcst])
    cx.op(dve, lambda: nc.vector.memset(onesb, 1.0), pwrites=[bcst])

    def vcol(c, j):
        return vecs[:, c, j:j + 1]

    def load_panel(pieces):
        i = wctr[0] % 2
        wctr[0] += 1
        first = True
        for src, co in pieces:
            n = src.shape[1]
            cx.dma(pool, wslot[i], WB[i][:, :, co:co + n], src.rearrange("(c p) n -> p c n", p=128),
                   writes=[bWB[i]] if first else (), pwrites=() if first else [bWB[i]])
            first = False
        return WB[i], bWB[i]

    def mm_group(out_ap, pairs, reads, bout):
        def emit():
            n = len(pairs)
            inst = None
            for i, (l, r) in enumerate(pairs):
                inst = nc.tensor.matmul(out_ap, lhsT=l, rhs=r, start=(i == 0), stop=(i == n - 1))
            return inst
        return cx.op(pe, emit, reads=reads, writes=[bout])

    def norm_to_A(src, gcol):
        cx.barrier()
        misc.reset()
        ht = [big.view(OFF_A2 + i * D * 4, [128, D], F32) for i in range(8)]
        yb = [misc.alloc([128, D], BF16) for _ in range(4)]
        junk = misc.alloc([128, D], BF16)
        st = misc.alloc([128, 2, 8], F32)
        bht = [Buf() for _ in range(8)]
        byb = [Buf() for _ in range(4)]
        bjunk = Buf()
        bst = [Buf(), Buf()]
        hs = [cx.slot(f"nh{i}") for i in range(8)]

        def load(g):
            for j in range(4):
                s_ = (g % 2) * 4 + j
                i = g * 4 + j
                cx.dma(sp, hs[s_], ht[s_], src[i * 128:(i + 1) * 128, :], writes=[bht[s_]])

        def stats(g):
            p = g % 2
            for j in range(4):
                s_ = p * 4 + j
                cx.op(act, lambda: nc.scalar.activation(out=junk, in_=ht[s_], func=AF.Square, accum_out=st[:, p, j:j + 1]),
                      reads=[bht[s_]], writes=[bjunk] if j == 0 else (), pwrites=[bst[p]] + ([] if j == 0 else [bjunk]))
            cx.op(act, lambda: nc.scalar.activation(out=st[:, p, 4:8], in_=st[:, p, 0:4], func=AF.Sqrt, scale=1.0 / D, bias=epsr),
                  reads=[bst[p], bcst], pwrites=[bst[p]])
            cx.op(dve, lambda: nc.vector.reciprocal(out=st[:, p, 4:8], in_=st[:, p, 4:8]), reads=[bst[p]], pwrites=[bst[p]])

        def scale(g):
            p = g % 2
            for j in range(4):
                s_ = p * 4 + j
                rs = st[:, p, 4 + j:5 + j]
                if j % 2 == 0:
                    cx.op(dve, lambda: nc.vector.tensor_scalar(out=yb[j], in0=ht[s_], scalar1=rs, scalar2=None, op0=ALU.mult),
                          reads=[bht[s_], bst[p]], writes=[byb[j]])
                else:
                    cx.op(act, lambda: nc.scalar.activation(out=yb[j], in_=ht[s_], func=AF.Copy, scale=rs),
                          reads=[bht[s_], bst[p]], writes=[byb[j]])

        def trans(g):
            for c in range(KC):
                pt, bp = psum()
                ptb = pt[:].bitcast(BF16)

                def emit():
                    inst = None
                    for j in range(4):
                        inst = nc.tensor.transpose(ptb[:, j * 128:(j + 1) * 128], yb[j][:, c * 128:(c + 1) * 128], identb)
                    return inst
                cx.op(pe, emit, reads=byb + [bcst], writes=[bp])
                cx.op(dve, lambda: nc.vector.tensor_scalar(out=A[:, c, g * 512:(g + 1) * 512], in0=ptb[:, 0:512],
                                                           scalar1=vcol(c, gcol), scalar2=None, op0=ALU.mult),
                      reads=[bp, bvecs], writes=[bAm[c][g]])

        load(0)
        if TG > 1:
            load(1)
        stats(0)
        for g in range(TG):
            if g + 1 < TG:
                stats(g + 1)
            scale(g)
            trans(g)
            if g + 2 < TG:
                load(g + 2)
        return bht

    def final_norm_out(src):
        cx.barrier()
        misc.reset()
        ht = [big.view(OFF_A2 + i * D * 4, [128, D], F32) for i in range(8)]
        ot = [big.view(OFF_A + i * D * 4, [128, D], F32) for i in range(8)]
        gb = misc.alloc([128, D], F32)
        junk = misc.alloc([128, D], BF16)
        st = misc.alloc([128, 2, 8], F32)
        bgb, bjunk = Buf(), Buf()
        bht = [Buf() for _ in range(8)]
        bot = [Buf() for _ in range(8)]
        bst = [Buf(), Buf()]
        hs = [cx.slot(f"fh{i}") for i in range(8)]
        os_ = [cx.slot(f"fo{i}") for i in range(8)]
        cx.dma(sp, cslot, gb, nfin_d[0:1, :].broadcast_to([128, D]), writes=[bgb])

        def load(g):
            for j in range(4):
                s_ = (g % 2) * 4 + j
                i = g * 4 + j
                cx.dma(sp, hs[s_], ht[s_], src[i * 128:(i + 1) * 128, :], writes=[bht[s_]])

        def stats(g):
            p = g % 2
            for j in range(4):
                s_ = p * 4 + j
                cx.op(act, lambda: nc.scalar.activation(out=junk, in_=ht[s_], func=AF.Square, accum_out=st[:, p, j:j + 1]),
                      reads=[bht[s_]], writes=[bjunk] if j == 0 else (), pwrites=[bst[p]] + ([] if j == 0 else [bjunk]))
            cx.op(act, lambda: nc.scalar.activation(out=st[:, p, 4:8], in_=st[:, p, 0:4], func=AF.Sqrt, scale=1.0 / D, bias=epsr),
                  reads=[bst[p], bcst], pwrites=[bst[p]])
            cx.op(dve, lambda: nc.vector.reciprocal(out=st[:, p, 4:8], in_=st[:, p, 4:8]), reads=[bst[p]], pwrites=[bst[p]])

        def scale(g):
            p = g % 2
            for j in range(4):
                s_ = p * 4 + j
                i = g * 4 + j
                cx.op(dve, lambda: nc.vector.scalar_tensor_tensor(out=ot[s_], in0=ht[s_], scalar=st[:, p, 4 + j:5 + j], in1=gb,
                                                                  op0=ALU.mult, op1=ALU.mult),
                      reads=[bht[s_], bst[p], bgb], writes=[bot[s_]])
                cx.dma(sp, os_[s_], y_d[i * 128:(i + 1) * 128, :], ot[s_], reads=[bot[s_]])

        load(0)
        if TG > 1:
            load(1)
        stats(0)
        for g in range(TG):
            if g + 1 < TG:
                stats(g + 1)
            scale(g)
            if g + 2 < TG:
                load(g + 2)

    epsr = cst.alloc([128, 1], F32)
    epsl = cst.alloc([128, 1], F32)
    cx.op(dve, lambda: nc.vector.memset(epsr, RMS_EPS), pwrites=[bcst])
    cx.op(dve, lambda: nc.vector.memset(epsl, LN_EPS), pwrites=[bcst])

    def dump(name, sb_ap, bufs):
        if name in dbg_d:
            cx.barrier()
            s = cx.slot("dbg")
            ev = cx.dma(sp, s, dbg_d[name], sb_ap, reads=bufs)
            sp.wait(ev)

    for l in range(L):
        hsrc = x_d if l == 0 else hA
        cx.barrier()
        cx.dma(sp, cslot, vecs, vecs_d[l].rearrange("p (c v) -> p c v", c=KC), writes=[bvecs])

        norm_to_A(hsrc, V_NMIX)
        if l == 0 and "A0" in dbg_d:
            cx.barrier()
            misc.reset()
            tmp = misc.alloc([128, 2048], F32)
            btmp = Buf()
            s = cx.slot("dbg")
            for c in range(KC):
                for t0 in range(0, T, 2048):
                    n = min(2048, T - t0)
                    cx.op(dve, lambda: nc.vector.tensor_copy(out=tmp[:, 0:n], in_=A[:, c, t0:t0 + n]), reads=bAm[c], writes=[btmp])
                    cx.dma(sp, s, dbg_d["A0"][:, c * T + t0:c * T + t0 + n], tmp[:, 0:n], reads=[btmp])

        cx.barrier()
        misc.reset()
        alr = misc.alloc([32, T], F32)
        wa2s = misc.alloc([32, 256], F32)
        balr, bwa2 = Buf(), Buf()
        MISC_GLA0 = misc.off
        cx.op(dve, lambda: nc.vector.memset(alr, 1.0), writes=[balr])
        wp, bw = load_panel([(w_in[l][:, OA:OA + 16], 0)])
        for tg in range(TG):
            pt, bp = psum()
            mm_group(pt[0:16, :], [(wp[:, c, 0:16], A[:, c, tg * 512:(tg + 1) * 512]) for c in range(KC)],
                     reads=[bw] + bA, bout=bp)
            cx.op(act, lambda: nc.scalar.copy(out=alr[0:16, tg * 512:(tg + 1) * 512], in_=pt[0:16, :]),
                  reads=[bp], pwrites=[balr])

        qe = big.view(OFF_A2, [128, 2, T], BF16)
        ke = big.view(OFF_A2 + 4 * T, [128, 2, T], BF16)
        kd = big.view(OFF_A2 + 8 * T, [128, NT, 256], BF16)
        vT = big.view(OFF_A2 + 8 * T + NT * 512, [128, NT, 512], BF16)
        srT = big.view(OFF_A2 + 8 * T + NT * 1536, [128, NT, 512], BF16)
        OGF0 = 8 * T + NT * 2560
        assert OGF0 <= ABYTES
        ogF_in_A2 = OGF0 + 2 * 4096 <= ABYTES
        if ogF_in_A2:
            ogF = [big.view(OFF_A2 + OGF0 + i * 4096, [128, 4, 512], BF16) for i in range(2)]
        bqe, bke, bkd, bvT, bsrT = Buf(), Buf(), Buf(), Buf(), Buf()
        misc.reset(MISC_GLA0)
        e_t = [misc.alloc([128, 2, 256], F32) for _ in range(2)]
        la = misc.alloc([128, 4, 256], F32)
        ebp = misc.alloc([128, 2, 512], F32)
        ebn = misc.alloc([128, 2, 512], F32)
        edk = misc.alloc([128, 4, 256], F32)
        kraw = misc.alloc([128, 2, 512], BF16)
        bkraw = Buf()
        dec = misc.alloc([128, 2, NT], F32)
        S = [misc.alloc([128, 2, 512], BF16) for _ in range(2)]
        attb = [misc.alloc([128, 128], BF16) for _ in range(2)]
        osq = misc.alloc([128, 512], BF16)
        ost = misc.alloc([128, 8], F32)
        ogT = [misc.alloc([128, 512], BF16) for _ in range(2)]
        if not ogF_in_A2:
            ogF = [misc.alloc([128, 4, 512], BF16) for _ in range(2)]
        be_t, bla, bebp, bebn, bedk, bdec = [Buf(), Buf()], Buf(), Buf(), Buf(), Buf(), Buf()
        bS, battb, bosq, bost, bogT, bogF = [Buf(), Buf()], [Buf(), Buf()], Buf(), [Buf(), Buf()], [Buf(), Buf()], [Buf(), Buf()]
        ogs = [cx.slot("og0"), cx.slot("og1")]
        bog_d = Buf()
        og_v = og_d.rearrange("(c p) t -> p c t", p=128)
        blk = 0
        for hd in range(NH):
            cx.dma(sp, cslot, wa2s[0:17, :], wa2[l][:, hd * 256:(hd + 1) * 256], writes=[bwa2])
            wp, bw = load_panel([(w_in[l][:, OQ + hd * DK:OQ + (hd + 1) * DK], 0),
                                 (w_in[l][:, OK_ + hd * DK:OK_ + (hd + 1) * DK], 256)])
            for tg in range(TG):
                tsl = slice(tg * 512, (tg + 1) * 512)
                for jj in range(2):
                    pt, bp = psum()

                    def emit():
                        inst = None
                        for j2 in range(2):
                            i = tg * 4 + jj * 2 + j2
                            inst = nc.tensor.matmul(pt[:, j2 * 256:(j2 + 1) * 256], lhsT=alr[0:17, i * 128:(i + 1) * 128],
                                                    rhs=wa2s[0:17, 0:256], start=True, stop=True)
                        return inst
                    cx.op(pe, emit, reads=[balr, bwa2], writes=[bp])
                    et = e_t[jj]
                    cx.op(act, lambda: nc.scalar.activation(out=et, in_=pt[:].rearrange("p (a b) -> p a b", a=2), func=AF.Exp, scale=-1.0),
                          reads=[bp], writes=[be_t[jj]])
                    cx.op(act, lambda: nc.scalar.activation(out=la[:, jj * 2:jj * 2 + 2, :], in_=et, func=AF.Ln, bias=1.0),
                          reads=[be_t[jj]], pwrites=[bla])
                for fc in range(2):
                    pt, bp = psum()

                    def emit():
                        inst = None
                        for j in range(4):
                            inst = nc.tensor.matmul(pt[:, j * 128:(j + 1) * 128], lhsT=la[:, j, fc * 128:(fc + 1) * 128],
                                                    rhs=trif, start=True, stop=True)
                        return inst
                    cx.op(pe, emit, reads=[bla, bcst], writes=[bp])
                    cx.op(act, lambda: nc.scalar.activation(out=ebp[:, fc, :], in_=pt[:], func=AF.Exp, scale=-1.0 / TAU),
                          reads=[bp], pwrites=[bebp])
                    cx.op(act, lambda: nc.scalar.activation(out=ebn[:, fc, :], in_=pt[:], func=AF.Exp, scale=1.0 / TAU),
                          reads=[bp], pwrites=[bebn])
                    cx.op(dve, lambda: nc.vector.tensor_copy(out=dec[:, fc, tg * 4:(tg + 1) * 4],
                                                             in_=ebp[:, fc, :].rearrange("p (j t) -> p j t", j=4)[:, :, 127]),
                          reads=[bebp], pwrites=[bdec])
                for jj in range(2):
                    pt, bp = psum()

                    def emit():
                        inst = None
                        for j2 in range(2):
                            inst = nc.tensor.matmul(pt[:, j2 * 256:(j2 + 1) * 256], lhsT=upf, rhs=la[:, jj * 2 + j2, :],
                                                    start=True, stop=True)
                        return inst
                    cx.op(pe, emit, reads=[bla, bcst], writes=[bp])
                    cx.op(act, lambda: nc.scalar.activation(out=edk[:, jj * 2:jj * 2 + 2, :], in_=pt[:].rearrange("p (a b) -> p a b", a=2),
                                                            func=AF.Exp, scale=-1.0 / TAU),
                          reads=[bp], pwrites=[bedk])
                for fc in range(2):
                    pt, bp = psum()
                    mm_group(pt[:], [(wp[:, c, fc * 128:(fc + 1) * 128], A[:, c, tsl]) for c in range(KC)], reads=[bw] + bA, bout=bp)
                    cx.op(dve, lambda: nc.vector.scalar_tensor_tensor(out=qe[:, fc, tsl], in0=pt[:], scalar=float(DK) ** -0.5,
                                                                      in1=ebp[:, fc, :], op0=ALU.mult, op1=ALU.mult),
                          reads=[bp, bebp], pwrites=[bqe])
                    pt, bp = psum()
                    mm_group(pt[:], [(wp[:, c, 256 + fc * 128:256 + (fc + 1) * 128], A[:, c, tsl]) for c in range(KC)], reads=[bw] + bA, bout=bp)
                    cx.op(dve, lambda: nc.vector.tensor_tensor(out=ke[:, fc, tsl], in0=pt[:], in1=ebn[:, fc, :], op=ALU.mult),
                          reads=[bp, bebn], pwrites=[bke])
                    cx.op(act, lambda: nc.scalar.copy(out=kraw[:, fc, :], in_=pt[:]), reads=[bp], pwrites=[bkraw])
                for jj in range(2):
                    pt, bp = psum()
                    ptb = pt[:].bitcast(BF16)

                    def emit():
                        inst = None
                        for j2 in range(2):
                            j = jj * 2 + j2
                            for fc in range(2):
                                inst = nc.tensor.transpose(ptb[:, j2 * 256 + fc * 128:j2 * 256 + (fc + 1) * 128],
                                                           kraw[:, fc, j * 128:(j + 1) * 128], identb)
                        return inst
                    cx.op(pe, emit, reads=[bkraw, bcst], writes=[bp])
                    i0_ = tg * 4 + jj * 2
                    cx.op(dve, lambda: nc.vector.tensor_tensor(out=kd[:, i0_:i0_ + 2, :], in0=ptb[:, 0:512].rearrange("p (a b) -> p a b", a=2),
                                                               in1=edk[:, jj * 2:jj * 2 + 2, :], op=ALU.mult),
                          reads=[bp, bedk], pwrites=[bkd])
            wp, bw = load_panel([(w_in[l][:, OV + hd * DV:OV + (hd + 1) * DV], 0)])
            for i in range(NT):
                pt, bp = psum()
                mm_group(pt[:], [(A[:, c, i * 128:(i + 1) * 128], wp[:, c, :]) for c in range(KC)], reads=[bw] + bA, bout=bp)
                cx.op(act, lambda: nc.scalar.copy(out=vT[:, i, :], in_=pt[:]), reads=[bp], pwrites=[bvT])
            wp, bw = load_panel([(w_in[l][:, OR + hd * DV:OR + (hd + 1) * DV], 0)])
            for i in range(NT):
                pt, bp = psum()
                mm_group(pt[:], [(A[:, c, i * 128:(i + 1) * 128], wp[:, c, :]) for c in range(KC)], reads=[bw] + bA, bout=bp)
                cx.op(act, lambda: nc.scalar.activation(out=srT[:, i, :], in_=pt[:], func=AF.Silu), reads=[bp], pwrites=[bsrT])
            rec = {}

            def r_att(n):
                nsl = slice(n * 128, (n + 1) * 128)
                pa, bpa = psum()
                mm_group(pa[:, 0:128], [(ke[:, fc, nsl], qe[:, fc, nsl]) for fc in range(2)], reads=[bke, bqe], bout=bpa)
                ab, bab = attb[n % 2], battb[n % 2]
                cx.op(dve, lambda: nc.vector.tensor_tensor(out=ab, in0=pa[:, 0:128], in1=trif, op=ALU.mult),
                      reads=[bpa, bcst], writes=[bab])

            def r_main(n):
                nsl = slice(n * 128, (n + 1) * 128)
                sc, sn = S[n % 2], S[(n + 1) % 2]
                bsc, bsn = bS[n % 2], bS[(n + 1) % 2]
                ab, bab = attb[n % 2], battb[n % 2]
                pss = []
                if n < NT - 1:
                    for fc in range(2):
                        p2, bp2 = psum()
                        mm_group(p2[:], [(kd[:, n, fc * 128:(fc + 1) * 128], vT[:, n, :])], reads=[bkd, bvT], bout=bp2)
                        pss.append((p2, bp2))
                po, bpo = psum()
                pairs = [(ab, vT[:, n, :])]
                rd = [bab, bvT]
                if n > 0:
                    pairs += [(qe[:, fc, nsl], sc[:, fc, :]) for fc in range(2)]
                    rd += [bqe, bsc]
                mm_group(po[:], pairs, reads=rd, bout=bpo)
                if n < NT - 1:
                    for fc in range(2):
                        p2, bp2 = pss[fc]
                        if n == 0:
                            cx.op(dve, lambda: nc.vector.tensor_copy(out=sn[:, fc, :], in_=p2[:]), reads=[bp2], pwrites=[bsn])
                        else:
                            cx.op(dve, lambda: nc.vector.scalar_tensor_tensor(out=sn[:, fc, :], in0=sc[:, fc, :], scalar=dec[:, fc, n:n + 1],
                                                                              in1=p2[:], op0=ALU.mult, op1=ALU.add),
                                  reads=[bp2, bsc, bdec], pwrites=[bsn])
                ss, rs = ost[:, 2 * (n % 2):2 * (n % 2) + 1], ost[:, 2 * (n % 2) + 1:2 * (n % 2) + 2]
                bo = bost[n % 2]
                cx.op(act, lambda: nc.scalar.activation(out=osq, in_=po[:], func=AF.Square, accum_out=ss),
                      reads=[bpo], writes=[bosq, bo])
                cx.op(act, lambda: nc.scalar.activation(out=rs, in_=ss, func=AF.Sqrt, scale=1.0 / DV, bias=epsr),
                      reads=[bo, bcst], pwrites=[bo])
                cx.op(dve, lambda: nc.vector.reciprocal(out=rs, in_=rs), reads=[bo], pwrites=[bo])
                og, bog = ogT[n % 2], bogT[n % 2]
                cx.op(dve, lambda: nc.vector.scalar_tensor_tensor(out=og, in0=po[:], scalar=rs, in1=srT[:, n, :],
                                                                  op0=ALU.mult, op1=ALU.mult),
                      reads=[bpo, bo, bsrT], writes=[bog])

            def r_tr(n, blk):
                og, bog = ogT[n % 2], bogT[n % 2]
                ptt, bpt = psum()
                ptb = ptt[:].bitcast(BF16)

                def emit():
                    inst = None
                    for ec in range(4):
                        inst = nc.tensor.transpose(ptb[:, ec * 128:(ec + 1) * 128], og[:, ec * 128:(ec + 1) * 128], identb)
                    return inst
                cx.op(pe, emit, reads=[bog, bcst], writes=[bpt])
                g = (blk // 4) % 2
                for ec in range(4):
                    cx.op(act, lambda: nc.scalar.activation(out=ogF[g][:, ec, (n % 4) * 128:(n % 4 + 1) * 128],
                                                            in_=ptb[:, ec * 128:(ec + 1) * 128], func=AF.Copy,
                                                            scale=vcol(hd * 4 + ec, V_GLAN)),
                          reads=[bpt, bvecs], pwrites=[bogF[g]])
                if n % 4 == 3:
                    tg = n // 4
                    cx.dma(sp, ogs[g], og_v[:, hd * 4:(hd + 1) * 4, tg * 512:(tg + 1) * 512], ogF[g],
                           reads=[bogF[g]], pwrites=[bog_d])

            r_att(0)
            for n in range(NT):
                if n + 1 < NT:
                    r_att(n + 1)
                r_main(n)
                if n >= 1:
                    r_tr(n - 1, blk + n - 1)
            r_tr(NT - 1, blk + NT - 1)
            blk += NT
        if l == 0 and "og" in dbg_d:
            cx.barrier()
            misc.reset()
            tb16 = misc.alloc([128, 2048], BF16)
            tmp = misc.alloc([128, 2048], F32)
            btmp, bt16 = Buf(), Buf()
            s = cx.slot("dbg")
            s2 = cx.slot("dbg2")
            for c in range(KC):
                cx.dma(sp, s2, tb16[:, 0:T], og_v[:, c, :], reads=[bog_d], writes=[bt16])
                cx.op(dve, lambda: nc.vector.tensor_copy(out=tmp[:, 0:T], in_=tb16[:, 0:T]), reads=[bt16], writes=[btmp])
                cx.dma(sp, s, dbg_d["og"][:, c * T:(c + 1) * T], tmp[:, 0:T], reads=[btmp])

        cx.barrier()
        misc.reset()
        PADL = 32
        ub = [misc.alloc([128, PADL + T], BF16) for _ in range(2)]
        NDV = 8
        dg = [misc.alloc([128, CW - NDV, 128], BF16) for _ in range(2)]
        sg = [misc.alloc([128, 512], F32) for _ in range(2)]
        accd = misc.alloc([128, T], F32)
        baccd = Buf()
        bub, bdg, bsg = [Buf(), Buf()], [Buf(), Buf()], [Buf(), Buf()]
        for s_ in range(2):
            cx.op(dve, lambda: nc.vector.memset(ub[s_][:, 0:PADL], 0.0), pwrites=[bub[s_]])
        sgk = [0]
        cpan = {}

        def glu(cc):
            cp, c2 = cc // 2, cc % 2
            if c2 == 0:
                cpan[cp] = load_panel([(w_in[l][:, OUV + cp * 256:OUV + (cp + 1) * 256], 0),
                                       (w_in[l][:, OUG + cp * 256:OUG + (cp + 1) * 256], 256)])
            wp, bw = cpan[cp]
            u, bu = ub[cc % 2], bub[cc % 2]
            for tg in range(TG):
                tsl = slice(tg * 512, (tg + 1) * 512)
                pg, bpg = psum()
                mm_group(pg[:], [(wp[:, c, 256 + c2 * 128:256 + (c2 + 1) * 128], A[:, c, tsl]) for c in range(KC)], reads=[bw] + bA, bout=bpg)
                pv, bpv = psum()
                mm_group(pv[:], [(wp[:, c, c2 * 128:(c2 + 1) * 128], A[:, c, tsl]) for c in range(KC)], reads=[bw] + bA, bout=bpv)
                sgt, bsgt = sg[sgk[0] % 2], bsg[sgk[0] % 2]
                sgk[0] += 1
                cx.op(act, lambda: nc.scalar.activation(out=sgt, in_=pg[:], func=AF.Sigmoid), reads=[bpg], writes=[bsgt])
                cx.op(dve, lambda: nc.vector.tensor_tensor(out=u[:, PADL + tg * 512:PADL + (tg + 1) * 512], in0=pv[:], in1=sgt, op=ALU.mult),
                      reads=[bpv, bsgt], pwrites=[bu])
            d_, bd_ = dg[cc % 2], bdg[cc % 2]
            for j in range(NDV, CW):
                cx.op(act, lambda: nc.scalar.activation(out=d_[:, j - NDV, :], in_=identb, func=AF.Copy, scale=vcol(cc, V_CW0 + j)),
                      reads=[bcst, bvecs], pwrites=[bd_])

        def conv_dve(cc):
            u, bu = ub[cc % 2], bub[cc % 2]
            for j in range(NDV):
                src = u[:, PADL - (CW - 1) + j:PADL - (CW - 1) + j + T]
                wj = vcol(cc, V_CW0 + j)
                if j == 0:
                    cx.op(dve, lambda: nc.vector.tensor_scalar(out=accd, in0=src, scalar1=wj, scalar2=vcol(cc, V_CONVB),
                                                               op0=ALU.mult, op1=ALU.add),
                          reads=[bu, bvecs], writes=[baccd])
                else:
                    cx.op(dve, lambda: nc.vector.scalar_tensor_tensor(out=accd, in0=src, scalar=wj, in1=accd, op0=ALU.mult, op1=ALU.add),
                          reads=[bu, bvecs], pwrites=[baccd])

        def conv_pe(cc):
            u, bu = ub[cc % 2], bub[cc % 2]
            d_, bd_ = dg[cc % 2], bdg[cc % 2]
            for tg in range(TG):
                pc, bpc = psum()
                mm_group(pc[:], [(d_[:, j - NDV, :], u[:, PADL - (CW - 1) + j + tg * 512:PADL - (CW - 1) + j + (tg + 1) * 512])
                                 for j in range(NDV, CW)],
                         reads=[bu, bd_], bout=bpc)
                cx.op(dve, lambda: nc.vector.tensor_tensor(out=A2[:, cc, tg * 512:(tg + 1) * 512], in0=pc[:],
                                                           in1=accd[:, tg * 512:(tg + 1) * 512], op=ALU.add),
                      reads=[bpc, baccd], pwrites=[bA2[cc]])

        glu(0)
        for cc in range(KC):
            conv_dve(cc)
            if cc + 1 < KC:
                glu(cc + 1)
            conv_pe(cc)

        misc.reset(misc.off)
        gst = [misc.alloc([128, 512], BF16) for _ in range(4)]
        bgst = [Buf() for _ in range(4)]
        gss = [cx.slot(f"gs{i}") for i in range(4)]
        bsig = [Buf("siga"), Buf("sigb")]
        gk = 0
        for which, (off, dst) in enumerate(((OGA, siga_d), (OGB, sigb_d))):
            for pn in range(4):
                wp, bw = load_panel([(w_in[l][:, off + pn * 512:off + (pn + 1) * 512], 0)])
                for fc in range(4):
                    for tg in range(TG):
                        tsl = slice(tg * 512, (tg + 1) * 512)
                        pt, bp = psum()
                        mm_group(pt[:], [(wp[:, c, fc * 128:(fc + 1) * 128], A[:, c, tsl]) for c in range(KC)], reads=[bw] + bA, bout=bp)
                        s = gk % 4
                        gk += 1
                        cx.op(act, lambda: nc.scalar.activation(out=gst[s], in_=pt[:], func=AF.Sigmoid), reads=[bp], writes=[bgst[s]])
                        r0 = (pn * 4 + fc) * 128
                        cx.dma(sp, gss[s], dst[r0:r0 + 128, tsl], gst[s], reads=[bgst[s]], pwrites=[bsig[which]])

        cx.barrier()
        misc.reset()
        sq = [misc.alloc([128, 512], BF16) for _ in range(4)]
        mean = [misc.alloc([128, 512], F32) for _ in range(2)]
        rstd = [misc.alloc([128, 512], F32) for _ in range(2)]
        t1 = [misc.alloc([128, 512], F32) for _ in range(4)]
        bsq, bmean, brstd, bt1 = [Buf() for _ in range(4)], [Buf(), Buf()], [Buf(), Buf()], [Buf() for _ in range(4)]
        ogs2 = cx.slot("ogl")
        for c in range(KC):
            cx.dma(sp, ogs2, A[:, c, :], og_d[c * 128:(c + 1) * 128, :], reads=[bog_d], writes=bAm[c])

        def ln_stats(tg):
            tsl = slice(tg * 512, (tg + 1) * 512)
            mn, rs_, bmn, brs = mean[tg % 2], rstd[tg % 2], bmean[tg % 2], brstd[tg % 2]
            p1, bp1 = psum()
            mm_group(p1[:], [(onesb, A2[:, c, tsl]) for c in range(KC)], reads=bA2 + [bcst], bout=bp1)
            p2, bp2 = psum()
            for c in range(KC):
                s_ = c % 4
                cx.op(act, lambda: nc.scalar.activation(out=sq[s_], in_=A2[:, c, tsl], func=AF.Square), reads=[bA2[c]], writes=[bsq[s_]])
                cx.op(pe, lambda: nc.tensor.matmul(p2[:], lhsT=onesb, rhs=sq[s_], start=(c == 0), stop=(c == KC - 1)),
                      reads=[bsq[s_], bcst], writes=[bp2] if c == 0 else (), pwrites=() if c == 0 else [bp2])
            cx.op(act, lambda: nc.scalar.mul(out=mn, in_=p1[:], mul=1.0 / D), reads=[bp1], writes=[bmn])
            cx.op(dve, lambda: nc.vector.tensor_tensor(out=rs_, in0=mn, in1=mn, op=ALU.mult), reads=[bmn], writes=[brs])
            cx.op(dve, lambda: nc.vector.scalar_tensor_tensor(out=rs_, in0=p2[:], scalar=1.0 / D, in1=rs_, op0=ALU.mult, op1=ALU.subtract),
                  reads=[bp2, brs], pwrites=[brs])
            cx.op(act, lambda: nc.scalar.activation(out=rs_, in_=rs_, func=AF.Sqrt, bias=epsl), reads=[brs, bcst], pwrites=[brs])
            cx.op(dve, lambda: nc.vector.reciprocal(out=rs_, in_=rs_), reads=[brs], pwrites=[brs])

        def ln_apply(tg):
            tsl = slice(tg * 512, (tg + 1) * 512)
            mn, rs_, bmn, brs = mean[tg % 2], rstd[tg % 2], bmean[tg % 2], brstd[tg % 2]
            for c in range(KC):
                s_ = c % 4
                cx.op(dve, lambda: nc.vector.tensor_tensor(out=t1[s_], in0=A2[:, c, tsl], in1=mn, op=ALU.subtract),
                      reads=[bA2[c], bmn], writes=[bt1[s_]])
                cx.op(dve, lambda: nc.vector.tensor_tensor(out=t1[s_], in0=t1[s_], in1=rs_, op=ALU.mult),
                      reads=[bt1[s_], brs], pwrites=[bt1[s_]])
                cx.op(act, lambda: nc.scalar.activation(out=A2[:, c, tsl], in_=t1[s_], func=AF.Silu,
                                                        scale=vcol(c, V_LNG), bias=vcol(c, V_LNB)),
                      reads=[bt1[s_], bvecs], pwrites=[bA2[c]])

        ln_stats(0)
        for tg in range(TG):
            if tg + 1 < TG:
                ln_stats(tg + 1)
            ln_apply(tg)
        if l == 0 and "ua" in dbg_d:
            cx.barrier()
            misc.reset()
            tmp = misc.alloc([128, 2048], F32)
            btmp = Buf()
            s = cx.slot("dbg")
            for c in range(KC):
                cx.op(dve, lambda: nc.vector.tensor_copy(out=tmp[:, 0:T], in_=A2[:, c, :]), reads=[bA2[c]], writes=[btmp])
                cx.dma(sp, s, dbg_d["ua"][:, c * T:(c + 1) * T], tmp[:, 0:T], reads=[btmp])

        def run_pipe(n, ld, body, pd):
            for k_ in range(min(pd, n)):
                ld(k_)
            for k_ in range(n):
                if k_ + pd < n:
                    ld(k_ + pd)
                body(k_)

        RG, PD = 4, 3
        cx.barrier()
        misc.reset()
        sgi = [misc.alloc([128, 512], BF16) for _ in range(RG)]
        tbo = [misc.alloc([128, 512], BF16) for _ in range(RG)]
        bsgi, btbo = [Buf() for _ in range(RG)], [Buf() for _ in range(RG)]
        sgis = [cx.slot(f"si{i}") for i in range(RG)]
        tbos = [cx.slot(f"to{i}") for i in range(RG)]
        btb_d = Buf("tb_d")
        tasks = [(pn, fc, tg) for pn in range(4) for fc in range(4) for tg in range(TG)]
        pan = {}

        def ld3(k):
            pn, fc, tg = tasks[k]
            ch = pn * 4 + fc
            s_ = k % RG
            cx.dma(sp, sgis[s_], sgi[s_], sigb_d[ch * 128:(ch + 1) * 128, tg * 512:(tg + 1) * 512], reads=[bsig[1]], writes=[bsgi[s_]])

        def body3(k):
            pn, fc, tg = tasks[k]
            if pn not in pan:
                pan[pn] = load_panel([(w_pw2[l][:, pn * 512:(pn + 1) * 512], 0)])
            wp, bw = pan[pn]
            ch = pn * 4 + fc
            tsl = slice(tg * 512, (tg + 1) * 512)
            s_ = k % RG
            pt, bp = psum()
            mm_group(pt[:], [(wp[:, c, fc * 128:(fc + 1) * 128], A2[:, c, tsl]) for c in range(KC)], reads=[bw] + bA2, bout=bp)
            cx.op(dve, lambda: nc.vector.scalar_tensor_tensor(out=tbo[s_], in0=pt[:], scalar=vcol(ch, V_BPW2), in1=sgi[s_],
                                                              op0=ALU.add, op1=ALU.mult),
                  reads=[bp, bsgi[s_], bvecs], writes=[btbo[s_]])
            cx.dma(sp, tbos[s_], tb_d[ch * 128:(ch + 1) * 128, tsl], tbo[s_], reads=[btbo[s_]], pwrites=[btb_d])
        run_pipe(len(tasks), ld3, body3, PD)

        cx.barrier()
        misc.reset()
        sai = [misc.alloc([128, 512], BF16) for _ in range(RG)]
        tbi = [misc.alloc([128, 512], BF16) for _ in range(RG)]
        m1 = [misc.alloc([128, 512], F32) for _ in range(2)]
        bsai, btbi, bm1 = [Buf() for _ in range(RG)], [Buf() for _ in range(RG)], [Buf(), Buf()]
        sais = [cx.slot(f"sa{i}") for i in range(RG)]
        tbis = [cx.slot(f"ti{i}") for i in range(RG)]
        pan = {}

        def ld2(k):
            pn, fc, tg = tasks[k]
            ch = pn * 4 + fc
            s_ = k % RG
            tsl = slice(tg * 512, (tg + 1) * 512)
            cx.dma(sp, sais[s_], sai[s_], siga_d[ch * 128:(ch + 1) * 128, tsl], reads=[bsig[0]], writes=[bsai[s_]])
            cx.dma(sp, tbis[s_], tbi[s_], tb_d[ch * 128:(ch + 1) * 128, tsl], reads=[btb_d], writes=[btbi[s_]])

        def body2(k):
            pn, fc, tg = tasks[k]
            if pn not in pan:
                pan[pn] = load_panel([(w_og[l][:, pn * 512:(pn + 1) * 512], 0)])
            wp, bw = pan[pn]
            ch = pn * 4 + fc
            tsl = slice(tg * 512, (tg + 1) * 512)
            s_ = k % RG
            s2 = k % 2
            pt, bp = psum()
            mm_group(pt[:], [(wp[:, c, fc * 128:(fc + 1) * 128], A[:, c, tsl]) for c in range(KC)], reads=[bw] + bA, bout=bp)
            cx.op(dve, lambda: nc.vector.tensor_tensor(out=m1[s2], in0=pt[:], in1=sai[s_], op=ALU.mult),
                  reads=[bp, bsai[s_]], writes=[bm1[s2]])
            cx.op(dve, lambda: nc.vector.tensor_tensor(out=A2[:, ch, tsl], in0=m1[s2], in1=tbi[s_], op=ALU.add),
                  reads=[bm1[s2], btbi[s_]], pwrites=[bA2[ch]])
        run_pipe(len(tasks), ld2, body2, PD)

        cx.barrier()
        misc.reset()
        hi = [misc.alloc([128, 512], F32) for _ in range(RG)]
        ho = [misc.alloc([128, 512], F32) for _ in range(RG)]
        bhi, bho = [Buf() for _ in range(RG)], [Buf() for _ in range(RG)]
        his = [cx.slot(f"hi{i}") for i in range(RG)]
        hos = [cx.slot(f"ho{i}") for i in range(RG)]
        bhB = Buf("hB")
        bhA = Buf("hA")
        tasks4 = [(pn, i) for pn in range(4) for i in range(NT)]
        pan = {}

        def ld4(k):
            pn, i = tasks4[k]
            s_ = k % RG
            cx.dma(sp, his[s_], hi[s_], hsrc[i * 128:(i + 1) * 128, pn * 512:(pn + 1) * 512], writes=[bhi[s_]])

        def body4(k):
            pn, i = tasks4[k]
            if pn not in pan:
                pan[pn] = load_panel([(w_out[l][:, pn * 512:(pn + 1) * 512], 0)])
            wp, bw = pan[pn]
            s_ = k % RG
            pt, bp = psum()
            mm_group(pt[:], [(A2[:, c, i * 128:(i + 1) * 128], wp[:, c, :]) for c in range(KC)], reads=[bw] + bA2, bout=bp)
            cx.op(dve, lambda: nc.vector.tensor_tensor(out=ho[s_], in0=pt[:], in1=hi[s_], op=ALU.add),
                  reads=[bp, bhi[s_]], writes=[bho[s_]])
            cx.dma(sp, hos[s_], hB[i * 128:(i + 1) * 128, pn * 512:(pn + 1) * 512], ho[s_], reads=[bho[s_]], pwrites=[bhB])
        run_pipe(len(tasks4), ld4, body4, PD)

        nbht = norm_to_A(hB, V_NFFN)

        misc.reset(misc.off)
        W7 = [big.view(OFF_A2, [128, HC, 512], BF16), big.view(OFF_A, [128, HC, 512], BF16)]
        assert HC * 512 * 2 + 2 * HC * 256 * 2 <= ABYTES + WBYTES
        AT = [big.view(OFF_A + HC * 512 * 2 + i * HC * 256 * 2, [128, HC, 256], BF16) for i in range(2)]
        bW7, bAT = [Buf(), Buf()], [Buf(), Buf()]
        w7s = [cx.slot("w70"), cx.slot("w71")]
        ats = [cx.slot("at0"), cx.slot("at1")]

        def load_w7(pn, extra=()):
            i = pn % 2
            half = HC // 2
            for hh in range(2):
                cx.dma(pool, w7s[i], W7[i][:, hh * half:(hh + 1) * half, :],
                       w_down[l][hh * half * 128:(hh + 1) * half * 128, pn * 512:(pn + 1) * 512].rearrange("(c p) n -> p c n", p=128),
                       writes=([bW7[i]] + list(extra)) if hh == 0 else (), pwrites=() if hh == 0 else [bW7[i]])
        sgf = [misc.alloc([128, 512], F32) for _ in range(2)]
        ao = [misc.alloc([128, 512], BF16) for _ in range(3)]
        bsgf, bao = [Buf(), Buf()], [Buf() for _ in range(3)]
        aos = [cx.slot(f"ao{i}") for i in range(3)]
        bact_d = Buf("act_d")
        k3 = 0
        for pn in range(HC // 2):
            wp, bw = load_panel([(w_gate[l][:, pn * 256:(pn + 1) * 256], 0), (w_up[l][:, pn * 256:(pn + 1) * 256], 256)])
            if pn == 3:
                load_w7(0, extra=nbht)
            for fc in range(2):
                hc = pn * 2 + fc
                for tg in range(TG):
                    tsl = slice(tg * 512, (tg + 1) * 512)
                    s = k3 % 3
                    s2 = k3 % 2
                    k3 += 1
                    pg, bpg = psum()
                    mm_group(pg[:], [(wp[:, c, fc * 128:(fc + 1) * 128], A[:, c, tsl]) for c in range(KC)], reads=[bw] + bAt(tg), bout=bpg)
                    pu, bpu = psum()
                    mm_group(pu[:], [(wp[:, c, 256 + fc * 128:256 + (fc + 1) * 128], A[:, c, tsl]) for c in range(KC)], reads=[bw] + bAt(tg), bout=bpu)
                    cx.op(act, lambda: nc.scalar.activation(out=sgf[s2], in_=pg[:], func=AF.Silu), reads=[bpg], writes=[bsgf[s2]])
                    cx.op(dve, lambda: nc.vector.tensor_tensor(out=ao[s], in0=pu[:], in1=sgf[s2], op=ALU.mult),
                          reads=[bpu, bsgf[s2]], writes=[bao[s]])
                    cx.dma(sp, aos[s], act_d[tg * 2:tg * 2 + 2, :, hc, :].rearrange("a p t -> p a t"),
                           ao[s].rearrange("p (a t) -> p a t", a=2), reads=[bao[s]], pwrites=[bact_d])

        cx.barrier(engs=cx.engs)
        misc.reset()
        hi = [misc.alloc([128, 512], F32) for _ in range(RG)]
        ho = [misc.alloc([128, 512], F32) for _ in range(RG)]
        bhi, bho = [Buf() for _ in range(RG)], [Buf() for _ in range(RG)]
        his = [cx.slot(f"hi{i}") for i in range(RG)]
        hos = [cx.slot(f"ho{i}") for i in range(RG)]

        NTT = T // 256
        tiles = [(pn, tt) for pn in range(4) for tt in range(NTT)]
        tasks7 = [(pn, tt, sub) for (pn, tt) in tiles for sub in range(2)]

        def load_at(idx):
            a = idx % 2
            cx.dma(sp, ats[a], AT[a], act_d[tiles[idx][1]], reads=[bact_d], writes=[bAT[a]])

        def ld7(k):
            pn, tt, sub = tasks7[k]
            i = tt * 2 + sub
            s_ = k % RG
            cx.dma(sp, his[s_], hi[s_], hB[i * 128:(i + 1) * 128, pn * 512:(pn + 1) * 512], reads=[bhB], writes=[bhi[s_]])

        def body7(k):
            pn, tt, sub = tasks7[k]
            idx = k // 2
            if sub == 0:
                if tt == 0 and pn + 1 < 4:
                    load_w7(pn + 1)
                if idx + 1 < len(tiles):
                    load_at(idx + 1)
            wv, bwv = W7[pn % 2], bW7[pn % 2]
            a = idx % 2
            i = tt * 2 + sub
            s_ = k % RG
            pt, bp = psum()
            mm_group(pt[:], [(AT[a][:, c, sub * 128:(sub + 1) * 128], wv[:, c, :]) for c in range(HC)], reads=[bwv, bAT[a]], bout=bp)
            cx.op(dve, lambda: nc.vector.tensor_tensor(out=ho[s_], in0=pt[:], in1=hi[s_], op=ALU.add),
                  reads=[bp, bhi[s_]], writes=[bho[s_]])
            cx.dma(sp, hos[s_], hA[i * 128:(i + 1) * 128, pn * 512:(pn + 1) * 512], ho[s_], reads=[bho[s_]], pwrites=[bhA])

        load_at(0)
        run_pipe(len(tasks7), ld7, body7, PD)
        cx.barrier(engs=cx.engs)

    if final_norm:
        final_norm_out(hA)
    else:
        pass
    cx.barrier(engs=[sp])
    return nc, stack


def _host_layout(inputs, L):
    f = lambda a: np.ascontiguousarray(np.asarray(a, dtype=np.float32))
    rows = []
    for l in range(L):
        r = [inputs["norm_mix"][l], inputs["gla_norm"][l], inputs["conv_b"][l], inputs["conv_norm_g"][l],
             inputs["conv_norm_b"][l], inputs["b_pw2"][l], inputs["norm_ffn"][l]]
        r += [inputs["conv_w"][l][j] for j in range(CW)]
        m = np.stack([np.asarray(v, dtype=np.float32) for v in r], axis=0)
        m = m.reshape(NVEC, KC, 128).transpose(2, 1, 0)
        rows.append(m.reshape(128, KC * NVEC))
    vecs = f(np.stack(rows, axis=0))
    wa2aug = f(np.concatenate([np.asarray(inputs["w_alpha2"], dtype=np.float32),
                               np.asarray(inputs["b_alpha2"], dtype=np.float32)[:, None, :]], axis=1))
    ident = np.eye(128, dtype=np.float32)
    tri = np.triu(np.ones((128, 128), dtype=np.float32))
    upper = np.tril(np.ones((128, 128), dtype=np.float32), -1)
    cstm = f(np.stack([ident, tri, upper], axis=0))
    return vecs, wa2aug, cstm


_CACHE = {}


def kernel(**inputs):
    x = np.asarray(inputs["x"], dtype=np.float32)
    B, T, _ = x.shape
    L = inputs["w_in"].shape[0]
    key = (T, L)
    if key not in _CACHE:
        _CACHE[key] = build_program(T, L)
    nc, _stack = _CACHE[key]
    vecs, wa2aug, cstm = _host_layout(inputs, L)
    f = lambda a: np.ascontiguousarray(np.asarray(a, dtype=np.float32))
    shared = {
        "w_in": f(inputs["w_in"]), "wa2aug": wa2aug, "w_o_gla": f(inputs["w_o_gla"]), "w_pw2": f(inputs["w_pw2"]),
        "w_out": f(inputs["w_out"]), "w_gate": f(inputs["w_gate"]), "w_up": f(inputs["w_up"]), "w_down": f(inputs["w_down"]),
        "vecs": vecs, "nfin": f(inputs["norm_final"]).reshape(1, D), "cst": cstm,
    }
    in_maps = [dict(shared, x=np.ascontiguousarray(x[b])) for b in range(B)]
    res = run_bass_kernel_spmd(nc, in_maps, core_ids=list(range(B)))
    return np.stack([np.asarray(r["y"], dtype=np.float32) for r in res.results], axis=0)
```
